# Optimizing a Trainium2 kernel written in Bass

```python
import math
import jax, jax.numpy as jnp
from jax import lax
import numpy as np

D_MODEL = 1024
BATCH = 4
SEQ = 8192
DEPTH = 1

CHUNK = 64
Q_BLOCK = 128
DA_HEADS = 4
DA_HEAD_DIM = 64
DA_WIDTH = DA_HEADS * 2 * DA_HEAD_DIM
SB_HEADS = 8
SB_HEAD_DIM = 64
SB_WIDTH = SB_HEADS * SB_HEAD_DIM
MIX_WIDTH = DA_WIDTH + SB_WIDTH
IN_WIDTH = 3 * DA_WIDTH + 3 * SB_WIDTH
REL_BUCKETS = 32
REL_MAX_DIST = 128
N_GROUPS = 4
EXPERTS_PER_GROUP = 8
N_EXPERTS = N_GROUPS * EXPERTS_PER_GROUP
TOP_K = 2
EXPERT_HIDDEN = 512
MOE_BLOCK = 256
EPS = 1e-6

kernel_name = "hymba_diff_stickbreak_hmoe_block"


def rms_norm(x, g):
    xf = x.astype(jnp.float32)
    y = xf * lax.rsqrt(jnp.mean(xf * xf, axis=-1, keepdims=True) + EPS)
    return (y * g.astype(jnp.float32)).astype(x.dtype)


def rel_bucket(rel):
    nb = REL_BUCKETS // 2
    max_exact = nb // 2
    base = jnp.where(rel > 0, nb, 0)
    n = jnp.abs(rel)
    nf = jnp.maximum(n, 1).astype(jnp.float32)
    large = max_exact + (jnp.log(nf / max_exact) / math.log(REL_MAX_DIST / max_exact)
                         * (nb - max_exact)).astype(jnp.int32)
    large = jnp.minimum(large, nb - 1)
    return base + jnp.where(n < max_exact, n, large)


def to_blocks(t):
    b, h, s, d = t.shape
    return jnp.transpose(t.reshape(b, h, s // Q_BLOCK, Q_BLOCK, d), (2, 0, 1, 3, 4))


def from_blocks(t):
    nblk, b, h, qb, d = t.shape
    return jnp.transpose(t, (1, 2, 0, 3, 4)).reshape(b, h, nblk * qb, d)


def diff_attention(q1, q2, k1, k2, v, rel_bias, lam):
    s_len = q1.shape[2]
    scale = q1.shape[-1] ** -0.5
    pos_k = jnp.arange(s_len, dtype=jnp.int32)

    def block(args):
        i, q1b, q2b = args
        pos_q = i * Q_BLOCK + jnp.arange(Q_BLOCK, dtype=jnp.int32)
        mask = (pos_k[None, :] // CHUNK) <= (pos_q[:, None] // CHUNK)
        bias = rel_bias[rel_bucket(pos_k[None, :] - pos_q[:, None])]
        bias = jnp.transpose(bias, (2, 0, 1)).astype(jnp.float32)

        def probs(qb, k):
            s = jnp.einsum('bhqd,bhkd->bhqk', qb, k).astype(jnp.float32) * scale + bias
            return jax.nn.softmax(jnp.where(mask, s, -jnp.inf), axis=-1)

        w = probs(q1b, k1) - lam * probs(q2b, k2)
        return jnp.einsum('bhqk,bhkd->bhqd', w.astype(v.dtype), v)

    nblk = s_len // Q_BLOCK
    out = lax.map(block, (jnp.arange(nblk, dtype=jnp.int32), to_blocks(q1), to_blocks(q2)))
    return from_blocks(out)


def stick_breaking(q, k, v):
    s_len = q.shape[2]
    scale = q.shape[-1] ** -0.5
    pos_k = jnp.arange(s_len, dtype=jnp.int32)

    def block(args):
        i, qb = args
        pos_q = i * Q_BLOCK + jnp.arange(Q_BLOCK, dtype=jnp.int32)
        causal = pos_k[None, :] < pos_q[:, None]
        z = jnp.einsum('bhqd,bhkd->bhqk', qb, k).astype(jnp.float32) * scale
        log_1m = jnp.where(causal, jax.nn.log_sigmoid(-z), 0.0)
        between = lax.cumsum(log_1m, axis=3, reverse=True) - log_1m
        a = jnp.where(causal, jnp.exp(jax.nn.log_sigmoid(z) + between), 0.0)
        return jnp.einsum('bhqk,bhkd->bhqd', a.astype(v.dtype), v)

    nblk = s_len // Q_BLOCK
    out = lax.map(block, (jnp.arange(nblk, dtype=jnp.int32), to_blocks(q)))
    return from_blocks(out)


def hier_moe(h, wg, bg, we, be, w1, w3, w2):
    bsz, s_len, d = h.shape
    n = bsz * s_len
    hf = h.reshape(n, d)
    pg = jax.nn.softmax((hf @ wg + bg).astype(jnp.float32), axis=-1)
    gsel = jnp.argmax(pg, axis=-1).astype(jnp.int32)
    pg_sel = jnp.max(pg, axis=-1)
    le = (hf @ we + be).astype(jnp.float32).reshape(n, N_GROUPS, EXPERTS_PER_GROUP)
    le = jnp.take_along_axis(le, gsel[:, None, None], axis=1)[:, 0]
    p_top, e_loc = lax.top_k(jax.nn.softmax(le, axis=-1), TOP_K)
    gate = pg_sel[:, None] * p_top / jnp.sum(p_top, axis=-1, keepdims=True)

    eid = (gsel[:, None] * EXPERTS_PER_GROUP + e_loc.astype(jnp.int32)).reshape(-1)
    tok = jnp.repeat(jnp.arange(n, dtype=jnp.int32), TOP_K)
    gflat = gate.reshape(-1)
    order = jnp.argsort(eid)
    s_eid, s_tok, s_gate = eid[order], tok[order], gflat[order]
    counts = jnp.zeros((N_EXPERTS,), jnp.int32).at[eid].add(1)
    starts = jnp.cumsum(counts) - counts
    padded = (counts + MOE_BLOCK - 1) // MOE_BLOCK * MOE_BLOCK
    pad_ends = jnp.cumsum(padded)
    pad_starts = pad_ends - padded
    dest = pad_starts[s_eid] + (jnp.arange(n * TOP_K, dtype=jnp.int32) - starts[s_eid])
    cap = n * TOP_K + N_EXPERTS * MOE_BLOCK
    tok_buf = jnp.full((cap,), n, jnp.int32).at[dest].set(s_tok)
    gate_buf = jnp.zeros((cap,), jnp.float32).at[dest].set(s_gate)
    nb = cap // MOE_BLOCK
    blk_e = jnp.minimum(jnp.searchsorted(pad_ends, jnp.arange(nb, dtype=jnp.int32) * MOE_BLOCK,
                                         side='right'), N_EXPERTS - 1).astype(jnp.int32)
    h_pad = jnp.concatenate([hf, jnp.zeros((1, d), hf.dtype)], axis=0)
    xs = h_pad[tok_buf].reshape(nb, MOE_BLOCK, d)

    def expert_block(args):
        xb, e = args
        return (jax.nn.silu(xb @ w1[e]) * (xb @ w3[e])) @ w2[e]

    ys = lax.map(expert_block, (xs, blk_e)).reshape(cap, d)
    ys = ys * gate_buf[:, None].astype(ys.dtype)
    out = jnp.zeros((n + 1, d), ys.dtype).at[tok_buf].add(ys)[:n]
    return out.reshape(bsz, s_len, d)


def setup_inputs(seed: int = 0) -> dict:
    key = jax.random.key(seed)
    ks = jax.random.split(key, 24)
    f32 = jnp.float32
    nrm = lambda k, shape, s: jax.random.normal(k, shape, f32) * s
    gain = lambda k, shape: 1.0 + 0.02 * jax.random.normal(k, shape, f32)
    return {
        "x": jax.random.normal(ks[0], (BATCH, SEQ, D_MODEL), f32),
        "g_attn": gain(ks[1], (DEPTH, D_MODEL)),
        "w_in": nrm(ks[2], (DEPTH, D_MODEL, IN_WIDTH), D_MODEL ** -0.5),
        "qn_g": gain(ks[3], (DEPTH, DA_HEAD_DIM)),
        "kn_g": gain(ks[4], (DEPTH, DA_HEAD_DIM)),
        "lam_q1": nrm(ks[5], (DEPTH, DA_HEAD_DIM), 0.1),
        "lam_k1": nrm(ks[6], (DEPTH, DA_HEAD_DIM), 0.1),
        "lam_q2": nrm(ks[7], (DEPTH, DA_HEAD_DIM), 0.1),
        "lam_k2": nrm(ks[8], (DEPTH, DA_HEAD_DIM), 0.1),
        "subln_g": gain(ks[9], (DEPTH, 2 * DA_HEAD_DIM)),
        "sb_out_g": gain(ks[10], (DEPTH, SB_HEAD_DIM)),
        "rel_bias": nrm(ks[11], (REL_BUCKETS, DA_HEADS), 0.5),
        "w_o": nrm(ks[12], (DEPTH, MIX_WIDTH, D_MODEL), MIX_WIDTH ** -0.5),
        "g_ffn": gain(ks[13], (DEPTH, D_MODEL)),
        "w_router_g": nrm(ks[14], (DEPTH, D_MODEL, N_GROUPS), D_MODEL ** -0.5),
        "b_router_g": nrm(ks[15], (DEPTH, N_GROUPS), 0.01),
        "w_router_e": nrm(ks[16], (DEPTH, D_MODEL, N_EXPERTS), D_MODEL ** -0.5),
        "b_router_e": nrm(ks[17], (DEPTH, N_EXPERTS), 0.01),
        "w1": nrm(ks[18], (DEPTH, N_EXPERTS, D_MODEL, EXPERT_HIDDEN), D_MODEL ** -0.5),
        "w3": nrm(ks[19], (DEPTH, N_EXPERTS, D_MODEL, EXPERT_HIDDEN), D_MODEL ** -0.5),
        "w2": nrm(ks[20], (DEPTH, N_EXPERTS, EXPERT_HIDDEN, D_MODEL), EXPERT_HIDDEN ** -0.5),
    }


def reference(x, g_attn, w_in, qn_g, kn_g, lam_q1, lam_k1, lam_q2, lam_k2, subln_g,
              sb_out_g, rel_bias, w_o, g_ffn, w_router_g, b_router_g, w_router_e,
              b_router_e, w1, w3, w2):
    bsz, s_len, _ = x.shape
    cut = np.cumsum([DA_WIDTH, DA_WIDTH, DA_WIDTH, SB_WIDTH, SB_WIDTH]).tolist()
    for l in range(DEPTH):
        lambda_init = 0.8 - 0.6 * math.exp(-0.3 * l)
        h = rms_norm(x, g_attn[l])
        proj = h @ w_in[l]
        dq, dk, dv, sq, sk, sv = jnp.split(proj, cut, axis=-1)

        dq = jnp.transpose(dq.reshape(bsz, s_len, DA_HEADS, 2, DA_HEAD_DIM), (3, 0, 2, 1, 4))
        dk = jnp.transpose(dk.reshape(bsz, s_len, DA_HEADS, 2, DA_HEAD_DIM), (3, 0, 2, 1, 4))
        dq = rms_norm(dq, qn_g[l])
        dk = rms_norm(dk, kn_g[l])
        dv = jnp.transpose(dv.reshape(bsz, s_len, DA_HEADS, 2 * DA_HEAD_DIM), (0, 2, 1, 3))
        lam = (jnp.exp(jnp.sum(lam_q1[l] * lam_k1[l]).astype(jnp.float32))
               - jnp.exp(jnp.sum(lam_q2[l] * lam_k2[l]).astype(jnp.float32)) + lambda_init)
        o_da = diff_attention(dq[0], dq[1], dk[0], dk[1], dv, rel_bias, lam)
        o_da = rms_norm(o_da, subln_g[l]) * (1.0 - lambda_init)
        o_da = jnp.transpose(o_da, (0, 2, 1, 3)).reshape(bsz, s_len, DA_WIDTH)

        to_heads = lambda t: jnp.transpose(t.reshape(bsz, s_len, SB_HEADS, SB_HEAD_DIM), (0, 2, 1, 3))
        o_sb = stick_breaking(to_heads(sq), to_heads(sk), to_heads(sv))
        o_sb = rms_norm(o_sb, sb_out_g[l])
        o_sb = jnp.transpose(o_sb, (0, 2, 1, 3)).reshape(bsz, s_len, SB_WIDTH)

        x = x + jnp.concatenate([o_da, o_sb], axis=-1) @ w_o[l]

        h2 = rms_norm(x, g_ffn[l])
        x = x + hier_moe(h2, w_router_g[l], b_router_g[l], w_router_e[l], b_router_e[l],
                         w1[l], w3[l], w2[l])
    return x
```

```python
import math
import numpy as np
import ml_dtypes
import concourse.bass as bass
import concourse.mybir as mybir
from concourse.bass_utils import run_bass_kernel_spmd

F32 = mybir.dt.float32
BF16 = mybir.dt.bfloat16
AF = mybir.ActivationFunctionType
ALU = mybir.AluOpType
AX = mybir.AxisListType

D = 1024
NCH = 8
IN_W = 3072
NEXP = 32
HID = 512
EPS = 1e-6
LAMBDA_INIT = 0.8 - 0.6 * math.exp(-0.3 * 0)
NEG = -30000.0
BIG = 1.0e4
SB_DUMMY_A = 0
SB_DUMMY_B = 1
MOE_B = 512


class Sem:
    def __init__(self, handle):
        self.h = handle
        self.count = 0


class KB:
    def __init__(self, nc, stack):
        self.nc = nc
        self.stack = stack
        self.eng = {"pe": nc.tensor, "act": nc.scalar, "dve": nc.vector, "pool": nc.gpsimd, "sp": nc.sync}
        self.esem = {k: self.new_sem("e_" + k) for k in self.eng}
        self.waited = {k: {} for k in self.eng}
        self.dma_sems = []
        self.nsem = 0

    def new_sem(self, name):
        return Sem(self.stack.enter_context(self.nc.semaphore(name)))

    def dsem(self, name):
        s = self.new_sem("d_" + name)
        self.dma_sems.append(s)
        return s

    def wait(self, e, deps):
        w = self.waited[e]
        for d in deps:
            if d is None:
                continue
            s, v = d
            if w.get(id(s), 0) < v:
                self.eng[e].wait_ge(s.h, v)
                w[id(s)] = v

    def op(self, e, fn, deps=(), sig=True):
        self.wait(e, deps)
        ins = fn(self.eng[e])
        if sig:
            s = self.esem[e]
            s.count += 1
            ins.then_inc(s.h, 1)
            return (s, s.count)
        return None

    def dma(self, q, sem, out, in_, deps=()):
        self.wait(q, deps)
        ins = self.eng[q].dma_start(out=out, in_=in_)
        sem.count += 16
        ins.then_inc(sem.h, 16)
        return (sem, sem.count)

    def drain_all(self):
        toks = [(s, s.count) for s in self.esem.values() if s.count] + [(s, s.count) for s in self.dma_sems if s.count]
        for e in self.eng:
            self.wait(e, toks)


def T(ap_or_handle):
    return ap_or_handle


def rel_bucket_np(rel):
    nb = 16
    max_exact = 8
    base = np.where(rel > 0, nb, 0)
    n = np.abs(rel)
    nf = np.maximum(n, 1).astype(np.float32)
    large = max_exact + (np.log(nf / np.float32(max_exact)) / np.float32(math.log(128 / max_exact))
                         * np.float32(nb - max_exact)).astype(np.int32)
    large = np.minimum(large, nb - 1)
    return base + np.where(n < max_exact, n, large)


def host_constants(hf, nb=48):
    c = {}
    eye = np.eye(128, dtype=np.float32)
    c["ident_bf"] = eye.astype(ml_dtypes.bfloat16)
    c["ident_f"] = eye
    c["jrev_bf"] = eye[::-1].copy().astype(ml_dtypes.bfloat16)
    j = np.arange(128)
    c["negT_bf"] = (-(j[:, None] >= j[None, :]).astype(np.float32)).astype(ml_dtypes.bfloat16)
    c["ones_bf"] = np.ones((128, 128), np.float32).astype(ml_dtypes.bfloat16)
    blk = np.zeros((128, 128), np.float32)
    blk[:64, :64] = 1.0
    blk[64:, 64:] = 1.0
    c["blk_ones_f"] = blk
    kk = np.arange(128)[:, None]
    qcol = np.arange(512)[None, :]
    sbm = np.zeros((8, 128, 512), np.float32)
    for s in range(8):
        kpos = 128 * s + kk
        qpos = 128 * 4 * hf + qcol
        sbm[s] = np.where(kpos < qpos, 0.0, NEG)
    c["sbmask"] = sbm.astype(ml_dtypes.bfloat16)
    dam = np.zeros((9, 128, 512), np.float32)
    for si in range(9):
        s = si - 1
        kpos = 128 * s + kk + 1024
        qpos = 128 * 4 * hf + qcol + 1024
        vis = (kpos // 64) <= (qpos // 64)
        dam[si] = np.where(vis, 0.0, NEG)[::-1]
    c["damask"] = dam
    R0 = 1023 - 512 * hf
    m = np.arange(1664)
    bk = rel_bucket_np((R0 - m).astype(np.int32))
    oh = np.zeros((32, 1664), np.float32)
    oh[bk, m] = 1.0
    oh[15, :] -= 1.0
    c["ohd"] = oh
    pp = np.arange(128, dtype=np.float32)[:, None]
    c["thr_c"] = np.tile((MOE_B * np.arange(nb, dtype=np.float32))[None, :], (128, 1))
    c["base13"] = (pp + 128.0 * np.arange(8, dtype=np.float32)[None, :]).astype(np.float32)
    c["strict_bf"] = (j[:, None] < j[None, :]).astype(np.float32).astype(ml_dtypes.bfloat16)
    return c


def build_nc(S):
    from contextlib import ExitStack
    NSLOT = S // 1024
    NOWN = S // 2
    NKB = S // 128
    NST = S // 512
    NOST = NOWN // 512
    NOT_ = NOWN // 128
    CAP = 2 * NOWN + NEXP * MOE_B
    NB = CAP // MOE_B
    nc = bass.Bass("TRN2", target_bir_lowering=False)

    def din(name, shape, dt=F32):
        return nc.dram_tensor(name, list(shape), dt, kind="ExternalInput")

    xb = din("xb", [S, D])
    xq = din("xq", [NOWN, D])
    g_attn = din("g_attn", [D]); w_in = din("w_in", [D, IN_W])
    qn_g = din("qn_g", [64]); kn_g = din("kn_g", [64])
    lam_q1 = din("lam_q1", [64]); lam_k1 = din("lam_k1", [64]); lam_q2 = din("lam_q2", [64]); lam_k2 = din("lam_k2", [64])
    subln_g = din("subln_g", [128]); sb_out_g = din("sb_out_g", [64])
    rel_bias = din("rel_bias", [32, 4])
    w_o = din("w_o", [D, D]); g_ffn = din("g_ffn", [D])
    w_rg = din("w_router_g", [D, 4]); b_rg = din("b_router_g", [4])
    w_re = din("w_router_e", [D, 32]); b_re = din("b_router_e", [32])
    w1 = din("w1", [NEXP, D, HID]); w3 = din("w3", [NEXP, D, HID]); w2 = din("w2", [NEXP, HID, D])
    c_ident_bf = din("ident_bf", [128, 128], BF16); c_ident_f = din("ident_f", [128, 128])
    c_jrev = din("jrev_bf", [128, 128], BF16); c_negT = din("negT_bf", [128, 128], BF16)
    c_ones = din("ones_bf", [128, 128], BF16); c_blk = din("blk_ones_f", [128, 128])
    c_sbmask = din("sbmask", [8, 128, 512], BF16); c_damask = din("damask", [9, 128, 512])
    c_ohd = din("ohd", [32, 1664])
    c_thr = din("thr_c", [128, NB]); c_base13 = din("base13", [128, 8]); c_strict = din("strict_bf", [128, 128], BF16)
    out = nc.dram_tensor("out", [NOWN, D], F32, kind="ExternalOutput")

    KT_da = nc.dram_tensor("KT_da", [4, 128, S], BF16)
    QT_da = nc.dram_tensor("QT_da", [4, 128, NOWN], BF16)
    V_da = nc.dram_tensor("V_da", [S, 4, 129], BF16)
    KT_sb = nc.dram_tensor("KT_sb", [4, 128, S], BF16)
    QT_sb = nc.dram_tensor("QT_sb", [4, 128, NOWN], BF16)
    V_sb = nc.dram_tensor("V_sb", [S, 512], BF16)
    U_sc = nc.dram_tensor("U_sc", [4, 1664], F32)
    X1_sc = nc.dram_tensor("X1_sc", [NOWN, D], F32)
    XS_sc = nc.dram_tensor("XS_sc", [CAP, D], BF16)
    HB_sc = nc.dram_tensor("HB_sc", [NOWN, D], BF16)
    YS_sc = nc.dram_tensor("YS_sc", [CAP, D], F32)

    def bc_row(t, n, parts=128, off=0):
        return bass.AP(t, off, [[0, parts], [1, n]])

    with ExitStack() as top:
        top.enter_context(nc.allow_non_contiguous_dma(reason="small strided constant / router weight loads"))
        kb = KB(nc, top)
        op, dma = kb.op, kb.dma
        sb = lambda name, shape, dt: top.enter_context(nc.sbuf_tensor(name, list(shape), dt))

        def rsqrt(out_ap, in_ap, n, deps):
            a_ = op("act", lambda e: e.activation(out=out_ap, in_=in_ap, func=AF.Ln, scale=1.0 / n, bias=EPS), deps)
            return op("act", lambda e: e.activation(out=out_ap, in_=out_ap, func=AF.Exp, scale=-0.5), [a_])

        ident_bf = sb("ident_bf_s", [128, 128], BF16); ident_f = sb("ident_f_s", [128, 128], F32)
        jrev = sb("jrev_s", [128, 128], BF16); negT = sb("negT_s", [128, 128], BF16)
        ones_bf = sb("ones_s", [128, 128], BF16); blk_f = sb("blk_s", [128, 128], F32)
        gsub_b = sb("gsub_b", [128, 128], F32); gsb_c = sb("gsb_c", [128, 1], F32)
        lamv = sb("lamv", [128, 4, 64], F32); lamt = sb("lamt", [128, 8], F32); neglam = sb("neglam", [128, 1], F32)
        gatt_c = sb("gatt_c", [128, NCH], F32)
        rb_s = sb("rb_s", [32, 4], F32)
        rbias_b = sb("rbias_b", [128, 36], F32)
        D12i = sb("D12i", [128, NOT_, 2], mybir.dt.int32)
        GT = sb("GT", [128, NOT_, 2], F32)
        idxwi = sb("idxwi", [128, NB], mybir.dt.int32)

        cs = kb.dsem("const")
        toks = []
        for dst, src in ((ident_bf, c_ident_bf), (ident_f, c_ident_f), (jrev, c_jrev), (negT, c_negT),
                         (ones_bf, c_ones), (blk_f, c_blk), (rb_s, rel_bias)):
            toks.append(dma("sp", cs, dst[:], src.ap()))
        for r in range(8):
            pass
        toks.append(dma("sp", cs, gsub_b[:], bc_row(subln_g, 128)))
        toks.append(dma("sp", cs, gsb_c[0:64, :], sb_out_g.ap().rearrange("(p o) -> p o", o=1)))
        toks.append(dma("sp", cs, gsb_c[64:128, :], sb_out_g.ap().rearrange("(p o) -> p o", o=1)))
        for i_, t_ in enumerate((lam_q1, lam_k1, lam_q2, lam_k2)):
            toks.append(dma("sp", cs, lamv[:, i_, :], bc_row(t_, 64)))
        toks.append(dma("sp", cs, gatt_c[:], g_attn.ap().rearrange("(c p) -> p c", p=128)))
        toks.append(dma("sp", cs, rbias_b[:, 0:4], bc_row(b_rg, 4)))
        toks.append(dma("sp", cs, rbias_b[:, 4:36], bc_row(b_re, 32)))
        cdone = toks[-1]

        t0 = op("dve", lambda e: e.tensor_scalar(out=gsub_b[:], in0=gsub_b[:], scalar1=1.0 - LAMBDA_INIT, scalar2=None, op0=ALU.mult), [cdone])
        t1 = op("dve", lambda e: e.tensor_tensor(out=lamv[:, 0, :], in0=lamv[:, 0, :], in1=lamv[:, 1, :], op=ALU.mult), [cdone])
        t2 = op("dve", lambda e: e.tensor_tensor(out=lamv[:, 2, :], in0=lamv[:, 2, :], in1=lamv[:, 3, :], op=ALU.mult), [cdone])
        t3 = op("dve", lambda e: e.tensor_reduce(out=lamt[:, 0:1], in_=lamv[:, 0, :], axis=AX.X, op=ALU.add), [t1])
        t4 = op("dve", lambda e: e.tensor_reduce(out=lamt[:, 1:2], in_=lamv[:, 2, :], axis=AX.X, op=ALU.add), [t2])
        t5 = op("act", lambda e: e.activation(out=lamt[:, 2:4], in_=lamt[:, 0:2], func=AF.Exp), [t3, t4])
        t6 = op("dve", lambda e: e.tensor_tensor(out=lamt[:, 4:5], in0=lamt[:, 3:4], in1=lamt[:, 2:3], op=ALU.subtract), [t5])
        t7 = op("dve", lambda e: e.tensor_scalar(out=neglam[:], in0=lamt[:, 4:5], scalar1=-LAMBDA_INIT, scalar2=None, op0=ALU.add), [t6])
        setup_done = t7

        mid = ExitStack()
        gk_b = mid.enter_context(nc.sbuf_tensor("gk_b", [128, 8, 64], F32))
        gq_b = mid.enter_context(nc.sbuf_tensor("gq_b", [128, 8, 64], F32))
        OT = mid.enter_context(nc.sbuf_tensor("OT", [128, NCH, NOWN], BF16))
        gsm = kb.dsem("gqk")
        gtk = None
        for r in range(8):
            gtk = dma("sp", gsm, gk_b[:, r, :], bc_row(kn_g, 64))
            gtk = dma("sp", gsm, gq_b[:, r, :], bc_row(qn_g, 64))
        t0q = op("dve", lambda e: e.tensor_scalar(out=gq_b[:], in0=gq_b[:], scalar1=0.125, scalar2=None, op0=ALU.mult), [gtk])
        with ExitStack() as ph:
            sbA = lambda name, shape, dt: ph.enter_context(nc.sbuf_tensor(name, list(shape), dt))
            psA = lambda name, shape, dt: ph.enter_context(nc.psum_tensor(name, list(shape), dt))
            W = sbA("W", [128, NCH, IN_W], BF16)
            tpA = [psA(f"tpA{i}", [128, NCH, 128], BF16) for i in range(2)]
            pj = [psA(f"pj{i}", [128, 512], F32) for i in range(4)]
            tpK = [psA(f"tpK{i}", [128, 4, 128], BF16) for i in range(2)]

            wst = [sbA(f"wst{i}", [128, 512], F32) for i in range(2)]
            wsem = [kb.dsem("w0"), kb.dsem("w1")]
            wfree = [None, None]
            Wr_tok = {}
            nw = 0
            for cb in (1, 2, 4, 5, 0, 3):
                wtok = None
                for c in range(NCH):
                    b_ = nw % 2; nw += 1
                    ld = dma("sp", wsem[b_], wst[b_][:], w_in.ap()[c * 128:(c + 1) * 128, cb * 512:(cb + 1) * 512], [wfree[b_]])
                    wtok = op("dve", lambda e, b_=b_, c=c, cb=cb: e.tensor_scalar(out=W[:, c, cb * 512:(cb + 1) * 512], in0=wst[b_][:], scalar1=gatt_c[:, c:c + 1], scalar2=None, op0=ALU.mult), [ld, cdone])
                    wfree[b_] = wtok
                Wr_tok[cb] = wtok
            xt = [sbA(f"xt{i}", [128, D], F32) for i in range(2)]
            junk2 = [sbA(f"junkA{i}", [128, D], BF16) for i in range(2)]
            stat2 = [sbA(f"statA{i}", [128, 8], F32) for i in range(2)]
            xn = [sbA(f"xn{i}", [128, D], BF16) for i in range(2)]
            xnT = [sbA(f"xnT{i}", [128, NCH, 512], BF16) for i in range(2)]
            ksq = [sbA(f"ksq{i}", [128, 512], F32) for i in range(2)]
            kst8 = [sbA(f"kst8{i}", [128, 16], F32) for i in range(2)]
            kn1 = [sbA(f"kn1{i}", [128, 512], F32) for i in range(2)]
            kn2 = [sbA(f"kn2{i}", [128, 512], BF16) for i in range(2)]
            KTst = [sbA(f"KTst{i}", [128, 4, 512], BF16) for i in range(2)]
            Vst = [sbA(f"Vst{i}", [128, 4, 4, 129], BF16) for i in range(2)]
            KSst = [sbA(f"KSst{i}", [128, 4, 512], BF16) for i in range(2)]
            VSst = [sbA(f"VSst{i}", [128, 4, 512], BF16) for i in range(2)]
            vinit = [op("dve", lambda e, i=i: e.memset(Vst[i][:], 1.0)) for i in range(2)]

            xsem = [kb.dsem("x0"), kb.dsem("x1")]
            stsems = {(k_, i_): kb.dsem(f"st{k_}{i_}") for k_ in ("kt", "ks", "v", "vs") for i_ in range(2)}
            st = {"xt_free": [None, None], "xn_free": [None, None], "xnT_free": [[], []], "tpA_free": [None, None],
                  "pj_free": [None] * 4, "pji": 0, "tpK_free": [None, None], "tpKi": 0, "kn2_free": [None, None], "kn2i": 0,
                  "stg_free": {}, "tile": 0, "stat_free": [None, None], "ksq_free": [None, None], "kst_free": [None, None], "kn1_free": [None, None]}

            def next_pj():
                i = st["pji"]; st["pji"] = (i + 1) % 4
                return i

            def proj_token_major(xT, tt, col0, deps):
                i = next_pj()
                tok = None
                for c in range(NCH):
                    tok = op("pe", lambda e, c=c, i=i: e.matmul(pj[i][:], lhsT=xT[:, c, tt * 128:(tt + 1) * 128], rhs=W[:, c, col0:col0 + 512], start=(c == 0), stop=(c == NCH - 1)),
                             deps + [st["pj_free"][i], Wr_tok[col0 // 512]] if c == 0 else [], sig=(c == NCH - 1))
                return i, tok

            def proj_feat_major(xT, col0, deps):
                i = next_pj()
                tok = None
                for c in range(NCH):
                    tok = op("pe", lambda e, c=c, i=i: e.matmul(pj[i][:], lhsT=W[:, c, col0:col0 + 128], rhs=xT[:, c, :], start=(c == 0), stop=(c == NCH - 1)),
                             deps + [st["pj_free"][i], Wr_tok[col0 // 512]] if c == 0 else [], sig=(c == NCH - 1))
                return i, tok

            def norm_stages(src, T_, xT, xT_free):
                ctx = {}
                res = {}

                def mk_a(tt):
                    def a():
                        n = st["tile"]; st["tile"] += 1
                        b_ = n % 2
                        row0 = T_ * 512 + tt * 128
                        ld = dma("pool", xsem[b_], xt[b_][:], src.ap()[row0:row0 + 128, :], [st["xt_free"][b_]])
                        sq = op("act", lambda e: e.activation(out=junk2[b_][:], in_=xt[b_][:], func=AF.Square, accum_out=stat2[b_][:, 0:1]), [ld, st["stat_free"][b_]])
                        r2 = rsqrt(stat2[b_][:, 2:3], stat2[b_][:, 0:1], D, [sq])
                        xo = op("dve", lambda e: e.tensor_scalar(out=xn[b_][:], in0=xt[b_][:], scalar1=stat2[b_][:, 2:3], scalar2=None, op0=ALU.mult), [r2, st["xn_free"][b_]])
                        st["xt_free"][b_] = xo
                        st["stat_free"][b_] = xo
                        ctx[tt] = (b_, xo)
                    return a

                def mk_b(tt):
                    def b():
                        b_, xo = ctx[tt]
                        tp = None
                        for c in range(NCH):
                            tp = op("pe", lambda e, c=c: e.transpose(tpA[b_][:, c, :], xn[b_][:, c * 128:(c + 1) * 128], ident_bf[:]),
                                    [xo, st["tpA_free"][b_], cdone] if c == 0 else [], sig=(c == NCH - 1))
                        st["xn_free"][b_] = tp
                        cp = op("dve", lambda e: e.tensor_copy(out=xT[:, :, tt * 128:(tt + 1) * 128], in_=tpA[b_][:]), [tp] + (xT_free if tt == 0 else []))
                        st["tpA_free"][b_] = cp
                        res["last"] = cp
                    return b
                return [mk_a(t) for t in range(4)], [mk_b(t) for t in range(4)], res

            def da_qk(xT, xready, T_, col0, g_b, dstT, ib):
                stg = KTst[ib]
                fr = st["stg_free"].get(("kt", ib))
                cps = []
                chain = {}

                def proj_chain(tt):
                    i, mm = proj_token_major(xT, tt, col0, [xready])
                    j = tt % 2
                    s1 = op("act", lambda e: e.activation(out=ksq[j][:], in_=pj[i][:], func=AF.Square), [mm, st["ksq_free"][j]])
                    s2 = op("dve", lambda e: e.tensor_reduce(out=kst8[j][:, 0:8], in_=ksq[j][:].rearrange("p (g d) -> p g d", d=64), axis=AX.X, op=ALU.add), [s1, st["kst_free"][j]])
                    st["ksq_free"][j] = s2
                    s4 = rsqrt(kst8[j][:, 8:16], kst8[j][:, 0:8], 64, [s2])
                    s5 = op("dve", lambda e: e.tensor_tensor(out=kn1[j][:].rearrange("p (g d) -> p g d", d=64), in0=pj[i][:].rearrange("p (g d) -> p g d", d=64),
                                                           in1=kst8[j][:, 8:16].unsqueeze(2).to_broadcast([128, 8, 64]), op=ALU.mult), [s4, st["kn1_free"][j]])
                    st["pj_free"][i] = s5
                    st["kst_free"][j] = s5
                    s6 = op("dve", lambda e: e.tensor_tensor(out=kn2[j][:], in0=kn1[j][:], in1=g_b[:].rearrange("p g d -> p (g d)"), op=ALU.mult), [s5, st["kn2_free"][j], t0, t0q])
                    st["kn1_free"][j] = s6
                    chain[tt] = s6

                def transp(tt):
                    j = tt % 2
                    k_ = st["tpKi"]; st["tpKi"] = (k_ + 1) % 2
                    tp = None
                    for h in range(4):
                        tp = op("pe", lambda e, h=h: e.transpose(tpK[k_][:, h, :], kn2[j][:, h * 128:(h + 1) * 128], ident_bf[:]),
                                [chain[tt], st["tpK_free"][k_]] if h == 0 else [], sig=(h == 3))
                    st["kn2_free"][j] = tp
                    cp = op("act", lambda e: e.copy(out=stg[:, :, tt * 128:(tt + 1) * 128], in_=tpK[k_][:]), [tp] + ([fr] if tt == 0 else []))
                    st["tpK_free"][k_] = cp
                    cps.append(cp)

                proj_chain(0); proj_chain(1); transp(0); proj_chain(2); transp(1); proj_chain(3)

                def tail():
                    transp(2); transp(3)
                    w = dma("sp", stsems[("kt", ib)], dstT.ap()[:, :, T_ * 512:(T_ + 1) * 512].rearrange("h p t -> p h t"), stg[:], [cps[-1]])
                    st["stg_free"][("kt", ib)] = w
                return tail

            def sb_qk(xT, xready, T_, col0, dstT, ib, scale):
                stg = KSst[ib]
                fr = st["stg_free"].get(("ks", ib))
                cp = None
                for p_ in range(4):
                    i, mm = proj_feat_major(xT, col0 + p_ * 128, [xready])
                    cp = op("dve", lambda e, i=i, p_=p_: e.tensor_scalar(out=stg[:, p_, :], in0=pj[i][:], scalar1=scale, scalar2=None, op0=ALU.mult), [mm] + ([fr] if p_ == 0 else []))
                    st["pj_free"][i] = cp
                w = dma("sp", stsems[("ks", ib)], dstT.ap()[:, :, T_ * 512:(T_ + 1) * 512].rearrange("h p t -> p h t"), stg[:], [cp])
                st["stg_free"][("ks", ib)] = w

            def da_v(xT, xready, T_, ib):
                stg = Vst[ib]
                fr = st["stg_free"].get(("v", ib))
                cp = None
                for tt in range(4):
                    i, mm = proj_token_major(xT, tt, 1024, [xready])
                    cp = op("act", lambda e, i=i, tt=tt: e.copy(out=stg[:, tt, :, 0:128], in_=pj[i][:].rearrange("p (h d) -> p h d", d=128)), [mm, vinit[ib]] + ([fr] if tt == 0 else []))
                    st["pj_free"][i] = cp
                w = dma("sp", stsems[("v", ib)], V_da.ap()[T_ * 512:(T_ + 1) * 512, :, :].rearrange("(tt p) h e -> p tt h e", p=128), stg[:], [cp])
                st["stg_free"][("v", ib)] = w

            def sb_v(xT, xready, T_, ib):
                stg = VSst[ib]
                fr = st["stg_free"].get(("vs", ib))
                cp = None
                for tt in range(4):
                    i, mm = proj_token_major(xT, tt, 2560, [xready])
                    cp = op("dve", lambda e, i=i, tt=tt: e.tensor_copy(out=stg[:, tt, :], in_=pj[i][:]), [mm] + ([fr] if tt == 0 else []))
                    st["pj_free"][i] = cp
                w = dma("sp", stsems[("vs", ib)], V_sb.ap()[T_ * 512:(T_ + 1) * 512, :].rearrange("(tt p) f -> p tt f", p=128), stg[:], [cp])
                st["stg_free"][("vs", ib)] = w

            jobs = [("kv", T_) for T_ in range(NST)] + [("q", T_) for T_ in range(NOST)]

            def prep(j):
                kind, T_ = jobs[j]
                ib = j % 2
                return norm_stages(xb if kind == "kv" else xq, T_, xnT[ib], st["xnT_free"][ib])

            a_st, b_st, res = prep(0)
            for t in range(4):
                a_st[t](); b_st[t]()
            for j, (kind, T_) in enumerate(jobs):
                ib = j % 2
                xready = res["last"]
                if j + 1 < len(jobs):
                    na, nb_, nres = prep(j + 1)
                else:
                    na = nb_ = [lambda: None] * 4
                    nres = None
                if kind == "kv":
                    tail = da_qk(xnT[ib], xready, T_, 512, gk_b, KT_da, ib)
                    na[0]()
                    da_v(xnT[ib], xready, T_, ib)
                    nb_[0](); na[1]()
                    tail()
                    nb_[1](); na[2]()
                    sb_qk(xnT[ib], xready, T_, 2048, KT_sb, ib, 1.0)
                    nb_[2](); na[3]()
                    sb_v(xnT[ib], xready, T_, ib)
                    nb_[3]()
                else:
                    tail = da_qk(xnT[ib], xready, T_, 0, gq_b, QT_da, ib)
                    na[0](); na[1]()
                    sb_qk(xnT[ib], xready, T_, 1536, QT_sb, ib, 0.125)
                    nb_[0](); nb_[1](); na[2]()
                    tail()
                    nb_[2](); na[3](); nb_[3]()
                st["xnT_free"][ib] = [(kb.esem["pe"], kb.esem["pe"].count)]
                res = nres
            kb.drain_all()
        nc.all_engine_barrier()

        with ExitStack() as ph:
            sbA = lambda name, shape, dt: ph.enter_context(nc.sbuf_tensor(name, list(shape), dt))
            psA = lambda name, shape, dt: ph.enter_context(nc.psum_tensor(name, list(shape), dt))
            Trev = sbA("Trev", [128, 4, 9, 512], BF16)
            Pb = [[sbA(f"P{m}_{i}", [128, 512], BF16) for i in range(3)] for m in range(2)]
            Ocp = sbA("Ocp", [128, 8, 129], F32)
            est = sbA("est", [128, 48], F32)
            eo1 = sbA("eo1", [128, 128], F32); ejk = sbA("ejk", [128, 128], F32)
            eo2 = [sbA(f"eo2_{i}", [128, 128], F32) for i in range(4)]
            eob = [sbA(f"eob{i}", [128, 128], BF16) for i in range(4)]
            Sb = [[psA(f"S{m}_{i}", [128, 512], F32) for i in range(2)] for m in range(2)]
            accO = [psA(f"accO{i}", [128, 512], F32) for i in range(3)]
            tpO = psA("tpO", [128, 8, 128], BF16)

            KT0 = sbA("KTd0", [128, S], BF16); QT0 = sbA("QTd0", [128, NOWN], BF16); Vd0 = sbA("Vd0", [128, NKB, 129], BF16)
            ldsem = [kb.dsem("daL0"), kb.dsem("daL1")]
            h0a = dma("sp", ldsem[0], KT0[:], KT_da.ap()[0])
            h0a = dma("sp", ldsem[0], QT0[:], QT_da.ap()[0])
            h0a = dma("sp", ldsem[0], Vd0[:], V_da.ap()[:, 0, :].rearrange("(k p) e -> p k e", p=128))
            tmpsc = ExitStack()
            sbT = lambda name, shape, dt: tmpsc.enter_context(nc.sbuf_tensor(name, list(shape), dt))
            ohd_s = sbT("ohd_s", [32, 1664], F32)
            u_s = sbT("u_s", [4, 1664], F32)
            NTB = 5
            tst = [sbT(f"tst{i}", [128, 512], F32) for i in range(NTB)]
            dmk9 = sbT("dmk9", [128, 9, 512], F32)
            bs = kb.dsem("bias")
            l0 = dma("sp", bs, ohd_s[:], c_ohd.ap())
            utok = None
            for q4 in range(4):
                mm = op("pe", lambda e, q4=q4: e.matmul(Sb[0][0][0:4, 0:416], lhsT=rb_s[:, :], rhs=ohd_s[:, q4 * 416:(q4 + 1) * 416], start=True, stop=True), [l0, cdone, utok])
                utok = op("dve", lambda e, q4=q4: e.tensor_copy(out=u_s[:, q4 * 416:(q4 + 1) * 416], in_=Sb[0][0][0:4, 0:416]), [mm])
            uw = dma("sp", bs, U_sc.ap(), u_s[:], [utok])
            kb.wait("sp", [uw])
            tfree = [None] * NTB
            bst = [kb.dsem(f"biasT{i}") for i in range(NTB)]
            dms = kb.dsem("dmk9")
            dml = dma("sp", dms, dmk9[:], c_damask.ap().rearrange("s p q -> p s q"))
            tlast = None
            n_ = 0
            for h in range(4):
                for si in range(9):
                    b_ = n_ % NTB; n_ += 1
                    off = 128 * (7 - (si - 1))
                    l1 = dma("sp", bst[b_], tst[b_][:], bass.AP(U_sc, h * 1664 + off, [[1, 128], [1, 512]]), [uw, tfree[b_]])
                    tlast = op("dve", lambda e, b_=b_, h=h, si=si: e.tensor_tensor(out=Trev[:, h, si, :], in0=tst[b_][:], in1=dmk9[:, si, :], op=ALU.add), [l1, dml])
                    tfree[b_] = tlast
            bias_ready = tlast
            tmpsc.close()
            KT = [KT0, sbA("KTd1", [128, S], BF16)]
            QT = [QT0, sbA("QTd1", [128, NOWN], BF16)]
            Vd = [Vd0, sbA("Vd1", [128, NKB, 129], BF16)]

            hfree = [None, bias_ready]
            acc_free = None
            tp_free = None
            S_free = [[utok, None], [None, None]]
            P_free = [[None] * 3, [None] * 3]
            un = 0
            eobi = 0
            ocp_free = None
            pending = []
            tpf = [None]; st2_prev = [None]; st3_prev = [None]

            def load_head(h):
                b_ = h % 2
                a = dma("sp", ldsem[b_], KT[b_][:], KT_da.ap()[h], [hfree[b_]])
                a = dma("sp", ldsem[b_], QT[b_][:], QT_da.ap()[h], [hfree[b_]])
                a = dma("sp", ldsem[b_], Vd[b_][:], V_da.ap()[:, h, :].rearrange("(k p) e -> p k e", p=128), [hfree[b_]])
                return a

            hl = {0: h0a}
            for h in range(4):
                b_ = h % 2
                if h + 1 < 4:
                    hl[h + 1] = load_head(h + 1)
                hready = hl[h]
                for i in range(NSLOT):
                    nkb = 8 * i + 8
                    q0 = i * 512
                    units = list(range(nkb))
                    stok = {}
                    ptok = {}
                    pvlast = None

                    def emit_qk(k_):
                        u_ = un + k_
                        sbk = u_ % 2
                        s_ = k_ - 8 * i
                        special = s_ >= -1
                        toks2 = []
                        for m in range(2):
                            r0 = 64 * m
                            tk = op("pe", lambda e, m=m, r0=r0, sbk=sbk: e.matmul(Sb[m][sbk][:], lhsT=KT[b_][r0:r0 + 64, k_ * 128:(k_ + 1) * 128], rhs=QT[b_][r0:r0 + 64, q0:q0 + 512], start=True, stop=not special),
                                    [hready, S_free[m][sbk], setup_done], sig=not special)
                            if special:
                                tk = op("pe", lambda e, m=m, sbk=sbk, s_=s_: e.matmul(Sb[m][sbk][:], lhsT=jrev[:], rhs=Trev[:, h, s_ + 1, :], start=False, stop=True), [bias_ready])
                            toks2.append(tk)
                        stok[k_] = toks2

                    def emit_exp(k_):
                        u_ = un + k_
                        sbk = u_ % 2
                        pb = u_ % 3
                        res = []
                        for m in range(2):
                            tk = op("act", lambda e, m=m, sbk=sbk, pb=pb: e.activation(out=Pb[m][pb][:], in_=Sb[m][sbk][:], func=AF.Exp), [stok[k_][m], P_free[m][pb]])
                            S_free[m][sbk] = tk
                            res.append(tk)
                        ptok[k_] = res

                    def emit_pv(k_):
                        nonlocal pvlast
                        u_ = un + k_
                        pb = u_ % 3
                        for m in range(2):
                            tk = None
                            for qb in range(4):
                                a_ = m * 4 + qb
                                tk = op("pe", lambda e, m=m, qb=qb, a_=a_, pb=pb: e.matmul(accO[a_ // 3][:, (a_ % 3) * 129:(a_ % 3) * 129 + 129], lhsT=Pb[m][pb][:, qb * 128:(qb + 1) * 128], rhs=Vd[b_][:, k_, :], start=(k_ == 0 and a_ % 3 == 0), stop=(k_ == nkb - 1 and (a_ % 3 == 2 or a_ == 7)), skip_group_check=True),
                                        [ptok[k_][m]] + ([acc_free] if k_ == 0 else []), sig=(qb == 3))
                            P_free[m][pb] = tk
                            pvlast = tk

                    emit_qk(0)
                    for r in range(nkb + 1):
                        if r + 1 < nkb:
                            emit_qk(r + 1)
                        if r < nkb:
                            emit_exp(r)
                        if r - 1 >= 0:
                            emit_pv(r - 1)
                        while pending and pending[0][0] <= r:
                            pending.pop(0)[1]()
                    while pending:
                        pending.pop(0)[1]()
                    un += nkb
                    cpt = None
                    for bk in range(3):
                        na = 3 if bk < 2 else 2
                        cpt = op("dve", lambda e, bk=bk, na=na: e.tensor_copy(out=Ocp[:, bk * 3:bk * 3 + na, :], in_=accO[bk][:, 0:na * 129].rearrange("p (a e) -> p a e", e=129)), [pvlast, ocp_free])
                    acc_free = cpt
                    e5s = []
                    e7 = None
                    for qb in range(4):
                        c0 = qb * 8
                        e1 = op("dve", lambda e, qb=qb, c0=c0: e.reciprocal(out=est[:, c0:c0 + 1], in_=Ocp[:, qb, 128:129]), [cpt, st3_prev[0]])
                        e2 = op("dve", lambda e, qb=qb, c0=c0: e.reciprocal(out=est[:, c0 + 1:c0 + 2], in_=Ocp[:, 4 + qb, 128:129]), [cpt])
                        e3 = op("dve", lambda e, c0=c0: e.tensor_scalar(out=est[:, c0 + 2:c0 + 3], in0=est[:, c0 + 1:c0 + 2], scalar1=neglam[:, 0:1], scalar2=None, op0=ALU.mult), [e2, setup_done])
                        e4 = op("dve", lambda e, qb=qb, c0=c0: e.tensor_scalar(out=eo1[:], in0=Ocp[:, qb, 0:128], scalar1=est[:, c0:c0 + 1], scalar2=None, op0=ALU.mult), [e1, e7])
                        e5 = op("dve", lambda e, qb=qb, c0=c0: e.scalar_tensor_tensor(out=eo2[qb][:], in0=Ocp[:, 4 + qb, 0:128], scalar=est[:, c0 + 2:c0 + 3], in1=eo1[:], op0=ALU.mult, op1=ALU.add), [e3, e4, st2_prev[0]])
                        e6 = op("dve", lambda e, qb=qb: e.tensor_tensor(out=ejk[:], in0=eo2[qb][:], in1=eo2[qb][:], op=ALU.mult), [e5])
                        e7 = op("dve", lambda e, qb=qb: e.tensor_reduce(out=est[:, 32 + qb:33 + qb], in_=ejk[:], axis=AX.X, op=ALU.add), [e6])
                        e5s.append(e5)
                    ocp_free = e5s[-1]
                    ctx = {"e7": e7}

                    def stage1(ctx=ctx):
                        ctx["e9"] = rsqrt(est[:, 36:40], est[:, 32:36], 128, [ctx["e7"]])

                    def stage2(ctx=ctx):
                        tpl = None
                        ep = None
                        for qb in range(4):
                            ep = op("dve", lambda e, qb=qb: e.scalar_tensor_tensor(out=eob[qb][:], in0=eo2[qb][:], scalar=est[:, 36 + qb:37 + qb], in1=gsub_b[:], op0=ALU.mult, op1=ALU.mult), [ctx["e9"], t0, tpf[0]])
                            tpl = op("pe", lambda e, qb=qb: e.transpose(tpO[:, qb, :], eob[qb][:], ident_bf[:]), [ep, tpf[0]])
                        st2_prev[0] = ep
                        ctx["tpl"] = tpl

                    def stage3(ctx=ctx, h=h, q0=q0):
                        tpf[0] = op("act", lambda e: e.copy(out=OT[:, h, q0:q0 + 512], in_=tpO[:, 0:4, :].rearrange("p a t -> p (a t)")), [ctx["tpl"]])
                        st3_prev[0] = tpf[0]

                    pending.extend([(8, stage1), (10, stage2), (12, stage3)])
                hfree[b_] = (kb.esem["pe"], kb.esem["pe"].count)
            while pending:
                pending.pop(0)[1]()
            kb.drain_all()
        nc.all_engine_barrier()

        with ExitStack() as ph:
            sbA = lambda name, shape, dt: ph.enter_context(nc.sbuf_tensor(name, list(shape), dt))
            psA = lambda name, shape, dt: ph.enter_context(nc.psum_tensor(name, list(shape), dt))
            sbm = sbA("sbm", [128, 8, 512], BF16)
            KT = [sbA(f"KTs{i}", [128, S], BF16) for i in range(2)]
            QT = [sbA(f"QTs{i}", [128, NOWN], BF16) for i in range(2)]
            Vs1 = sbA("Vs", [128, NKB, 2, 128], BF16)
            Vs = [Vs1, Vs1]
            Eb = [sbA(f"Eb{i}", [128, 512], F32) for i in range(3)]
            Lb = [sbA(f"Lb{i}", [128, 512], BF16) for i in range(3)]
            Ab = [sbA(f"Ab{i}", [128, 512], F32) for i in range(3)]
            Pb2 = [sbA(f"Pa{i}", [128, 512], BF16) for i in range(3)]
            Rc = [sbA(f"Rc{i}", [128, 512], F32) for i in range(3)]
            sq_s = sbA("sq_s", [128, 512], F32)
            rs_s = sbA("rs_s", [128, 512], F32)
            Zb = [psA(f"Z{i}", [128, 512], F32) for i in range(4)]
            Racc = psA("Racc", [128, 512], F32)
            Oacc = [psA(f"Oacc{i}", [128, 512], F32) for i in range(2)]
            ssqb = psA("ssqb", [128, 512], F32)

            ztC = sbA("ztC", [128, 2048], BF16)
            zm = op("pool", lambda e: e.memset(ztC[:], 0.0))
            zsem = kb.dsem("zfill")
            zstate = {"next": 0, "tok": None}
            rows_per = 128 * 2

            def zero_fill_step(dep=None):
                r_ = zstate["next"]
                if r_ >= CAP:
                    return
                zstate["tok"] = dma("pool", zsem, XS_sc.ap()[r_:r_ + rows_per, :].rearrange("(p a) d -> p (a d)", p=128), ztC[:], [zm, dep])
                zstate["next"] = r_ + rows_per
            ms = kb.dsem("sbm")
            mload = dma("sp", ms, sbm[:], c_sbmask.ap().rearrange("s p q -> p s q"))
            vz0 = op("dve", lambda e: e.memset(Vs1[:], 0.0))
            vz = [vz0, vz0]
            vsem = kb.dsem("sbV")
            vfree = [None]
            vl = {}
            ldsem = [kb.dsem("sbL0"), kb.dsem("sbL1")]
            pfree = [None, None]

            def load_pair(p_):
                b_ = p_ % 2
                a = dma("sp", ldsem[b_], KT[b_][:], KT_sb.ap()[p_], [pfree[b_]])
                a = dma("sp", ldsem[b_], QT[b_][:], QT_sb.ap()[p_], [pfree[b_]])
                return a

            def load_v(p_):
                a = dma("sp", vsem, Vs1[:, :, 0, 0:64], V_sb.ap()[:, p_ * 128:p_ * 128 + 64].rearrange("(k p) e -> p k e", p=128), [vfree[0], vz0])
                a = dma("sp", vsem, Vs1[:, :, 1, 64:128], V_sb.ap()[:, p_ * 128 + 64:p_ * 128 + 128].rearrange("(k p) e -> p k e", p=128), [vfree[0], vz0])
                vl[p_] = a

            units = []
            for p_ in range(4):
                for i in range(NSLOT):
                    nkb = 8 * i + 8
                    for hh in range(2):
                        for k_ in range(nkb - 1, -1, -1):
                            units.append(dict(p=p_, i=i, hh=hh, k=k_, first=(k_ == nkb - 1), ofirst=(hh == 0 and k_ == nkb - 1), olast=(hh == 1 and k_ == 0)))
            NU = len(units)
            pl = {0: load_pair(0), 1: load_pair(1)}
            load_v(0)
            tk = {}
            Z_free = [None] * 4; E_free = [None] * 3; L_free = [[None, None] for _ in range(3)]; A_free = [None] * 3; P_free = [None] * 3
            O_free = [None, None]
            state = {"s7_prev": None, "s5_prev": None, "ssq_free": None, "sq_free": None, "rs_free": None, "ogrp": 0}

            def S1(u):
                U = units[u]; b_ = U["p"] % 2; z = u % 4; r0 = 64 * U["hh"]; k_ = U["k"]; q0 = U["i"] * 512
                s_ = k_ - 8 * U["i"]
                special = s_ >= 0
                t_ = op("pe", lambda e: e.matmul(Zb[z][:], lhsT=KT[b_][r0:r0 + 64, k_ * 128:(k_ + 1) * 128], rhs=QT[b_][r0:r0 + 64, q0:q0 + 512], start=True, stop=not special),
                        [pl[U["p"]], Z_free[z]], sig=not special)
                if special:
                    t_ = op("pe", lambda e: e.matmul(Zb[z][:], lhsT=ident_bf[:], rhs=sbm[:, s_, :], start=False, stop=True), [mload, cdone])
                tk[("s1", u)] = t_

            def S2(u):
                z = u % 3
                tk[("s2", u)] = op("act", lambda e: e.activation(out=Eb[z][:], in_=Zb[u % 4][:], func=AF.Exp), [tk[("s1", u)], E_free[z]])

            def S3(u):
                z = u % 3
                t_ = op("act", lambda e: e.activation(out=Lb[z][:], in_=Eb[z][:], func=AF.Ln, bias=1.0), [tk[("s2", u)]] + L_free[z])
                E_free[z] = t_
                tk[("s3", u)] = t_

            def S4(u):
                z = u % 3
                t_ = op("pe", lambda e: e.matmul(Zb[u % 4][:], lhsT=negT[:], rhs=Lb[z][:], start=False, stop=True, skip_group_check=True), [tk[("s3", u)], cdone])
                L_free[z][0] = t_
                tk[("s4", u)] = t_

            def S5a(u):
                z = u % 3
                U = units[u]
                if U["first"]:
                    tk[("s5a", u)] = None
                    return
                tk[("s5a", u)] = op("dve", lambda e: e.tensor_copy(out=Rc[z][:], in_=Racc[:]), [state["s7_prev"], tk.get(("s5", u - 3))])

            def S5(u):
                z = u % 3
                U = units[u]
                if U["first"]:
                    t_ = op("dve", lambda e: e.tensor_copy(out=Ab[z][:], in_=Zb[u % 4][:]), [tk[("s4", u)], A_free[z]])
                else:
                    t_ = op("dve", lambda e: e.tensor_tensor(out=Ab[z][:], in0=Zb[u % 4][:], in1=Rc[z][:], op=ALU.subtract), [tk[("s4", u)], A_free[z], tk[("s5a", u)]])
                Z_free[u % 4] = t_
                tk[("s5", u)] = t_

            def S7(u):
                z = u % 3
                U = units[u]
                t_ = op("pe", lambda e: e.matmul(Racc[:], lhsT=ones_bf[:], rhs=Lb[z][:], start=U["first"], stop=True, skip_group_check=True), [tk[("s5a", u)], tk[("s3", u)]])
                L_free[z][1] = t_
                state["s7_prev"] = t_
                tk[("s7", u)] = t_

            def S6(u):
                z = u % 3
                t_ = op("act", lambda e: e.activation(out=Pb2[z][:], in_=Ab[z][:], func=AF.Exp), [tk[("s5", u)], P_free[z]])
                A_free[z] = t_
                tk[("s6", u)] = t_

            def S8(u):
                z = u % 3
                U = units[u]; b_ = U["p"] % 2; og = state["ogrp"] % 2
                t_ = op("pe", lambda e: e.matmul(Oacc[og][:], lhsT=Vs[b_][:, U["k"], U["hh"], :], rhs=Pb2[z][:], start=U["ofirst"], stop=U["olast"]),
                        [tk[("s6", u)], vl[U["p"]]] + ([O_free[og]] if U["ofirst"] else []))
                P_free[z] = t_
                if U["olast"]:
                    epilogue(U, og, t_)
                    state["ogrp"] += 1
                    if U["i"] == NSLOT - 1:
                        pfree[b_] = t_
                        vfree[0] = t_
                        if U["p"] + 1 < 4:
                            load_v(U["p"] + 1)
                        if U["p"] + 2 < 4:
                            pl[U["p"] + 2] = load_pair(U["p"] + 2)

            def epilogue(U, og, done):
                q0 = U["i"] * 512
                a1 = op("act", lambda e: e.activation(out=sq_s[:], in_=Oacc[og][:], func=AF.Square), [done, state["sq_free"]])
                a2 = op("pe", lambda e: e.matmul(ssqb[:], lhsT=blk_f[:], rhs=sq_s[:], start=True, stop=True), [a1, state["ssq_free"], cdone])
                state["sq_free"] = a2
                a3 = op("act", lambda e: e.activation(out=rs_s[:], in_=ssqb[:], func=AF.Ln, scale=1.0 / 64, bias=EPS), [a2, state["rs_free"]])
                state["ssq_free"] = a3
                a4 = op("act", lambda e: e.activation(out=rs_s[:], in_=rs_s[:], func=AF.Exp, scale=-0.5), [a3])
                a5 = op("dve", lambda e: e.scalar_tensor_tensor(out=OT[:, 4 + U["p"], q0:q0 + 512], in0=Oacc[og][:], scalar=gsb_c[:, 0:1], in1=rs_s[:], op0=ALU.mult, op1=ALU.mult), [a4, cdone])
                state["rs_free"] = a5
                O_free[og] = a5

            def dummy():
                op("pe", lambda e: e.matmul(ssqb[:], lhsT=ones_bf[:], rhs=sbm[:, 0, :], start=True, stop=True, skip_group_check=True), [state["ssq_free"], mload, cdone], sig=False)

            S1(0)
            zevery = max(1, (NU - 40) // (CAP // rows_per + 1))
            for r in range(NU + 4):
                if r >= 20 and r % zevery == 0:
                    zero_fill_step(tk.get(("s2", min(r - 1, NU - 1))))
                if 0 <= r < NU: S2(r)
                if 0 <= r - 1 < NU: S3(r - 1)
                if 0 <= r - 3 < NU: S8(r - 3)
                if 0 <= r - 2 < NU: S7(r - 2)
                if 0 <= r - 2 < NU: S6(r - 2)
                if 0 <= r + 1 < NU: S1(r + 1)
                for _ in range(SB_DUMMY_A):
                    dummy()
                if 0 <= r - 1 < NU:
                    S5a(r - 1)
                    S4(r - 1)
                    S5(r - 1)
                for _ in range(SB_DUMMY_B):
                    dummy()
            while zstate["next"] < CAP:
                zero_fill_step()
            ztok = zstate["tok"]
            kb.drain_all()
        nc.all_engine_barrier()

        I32 = mybir.dt.int32
        IOA = bass.IndirectOffsetOnAxis
        with ExitStack() as ph:
            sbA = lambda name, shape, dt: ph.enter_context(nc.sbuf_tensor(name, list(shape), dt))
            psA = lambda name, shape, dt: ph.enter_context(nc.psum_tensor(name, list(shape), dt))
            gffn_b = sbA("gffn_b", [128, D], F32)
            gfs = kb.dsem("gffn")
            gffn_ready = dma("sp", gfs, gffn_b[:], bc_row(g_ffn, D))
            Wo = sbA("Wo", [128, NCH, D], BF16)
            Wr = sbA("Wr", [128, NCH, 36], F32)
            wst = [sbA(f"wstD{i}", [128, D], F32) for i in range(2)]
            xr = [sbA(f"xr{i}", [128, D], F32) for i in range(2)]
            x1 = [sbA(f"x1_{i}", [128, D], F32) for i in range(2)]
            junk = sbA("junkD", [128, D], BF16)
            hf2 = [sbA(f"hf_{i}", [128, D], F32) for i in range(2)]
            statf = [sbA(f"statf{i}", [128, 4], F32) for i in range(2)]
            junkf = [sbA(f"junkf{i}", [128, D], BF16) for i in range(2)]
            hb_ = [sbA(f"hb{i}", [128, D], BF16) for i in range(2)]
            NHB = 8
            hbl = [sbA(f"hbl{i}", [128, D], BF16) for i in range(NHB)]
            hTf = sbA("hTf", [128, NCH, 128], F32)
            rt = sbA("rt", [128, 256], F32)
            stat = sbA("statD", [128, 16], F32)
            G_all = sbA("G_all", [128, NOT_, 32], F32)
            SELb = sbA("SELb", [128, NOT_, 32], BF16)
            SELf = sbA("SELf", [128, NOT_, 32], F32)
            SEL1 = sbA("SEL1", [128, NOT_, 32], F32)
            POS = sbA("POS", [128, NOT_, 32], F32)
            strict_s = sbA("strict_s", [128, 128], BF16)
            thr_s = sbA("thr_s", [128, NB], F32)
            base_s = sbA("base_s", [128, 8], F32)
            big = sbA("bigtmp", [128, max(NOT_, NB) * 32], F32)
            pA = [psA(f"pA{i}", [128, 512], F32) for i in range(4)]
            tpF = psA("tpF", [128, 512], F32)
            pR = psA("pR", [128, 512], F32)

            cs2 = kb.dsem("constD")
            a = dma("sp", cs2, strict_s[:], c_strict.ap())
            a = dma("sp", cs2, thr_s[:], c_thr.ap())
            a = dma("sp", cs2, base_s[:], c_base13.ap())
            cD = a

            wsem = [kb.dsem("wD0"), kb.dsem("wD1")]
            wfree = [None, None]
            wt = None
            for c in range(NCH):
                b_ = c % 2
                ld = dma("sp", wsem[b_], wst[b_][:], w_o.ap()[c * 128:(c + 1) * 128, :], [wfree[b_]])
                wt = op("dve", lambda e, b_=b_, c=c: e.tensor_copy(out=Wo[:, c, :], in_=wst[b_][:]), [ld])
                wfree[b_] = wt
            Wo_ready = wt
            rs_ = kb.dsem("wr")
            a = dma("sp", rs_, Wr[:, :, 0:4], w_rg.ap().rearrange("(c p) n -> p c n", p=128))
            a = dma("sp", rs_, Wr[:, :, 4:36], w_re.ap().rearrange("(c p) n -> p c n", p=128))
            Wr_ready = a

            xsem = [kb.dsem("xr0"), kb.dsem("xr1")]
            osem = [kb.dsem("x1w0"), kb.dsem("x1w1")]
            x1w_toks = [None, None]
            xr_free = [None, None]; x1_free = [None, None]
            pA_free = [None] * 4
            dst = {"tpF_free": None, "hf_free": None, "hTf_free": None, "rt_free": None, "stat_free": None}
            hbsem = [kb.dsem("hbw0"), kb.dsem("hbw1")]
            hbw = [None, None]
            tile_ctx = {}
            hf_free = [None, None]; statf_free = [None, None]

            def front(n):
                b_ = n % 2
                r0 = n * 128
                ld = dma("sp", xsem[b_], xr[b_][:], xq.ap()[r0:r0 + 128, :], [xr_free[b_]])
                hs = []
                for half in range(2):
                    i = (2 * n + half) % 4
                    mm = None
                    for c in range(NCH):
                        mm = op("pe", lambda e, c=c, i=i, half=half: e.matmul(pA[i][:], lhsT=OT[:, c, r0:r0 + 128], rhs=Wo[:, c, half * 512:(half + 1) * 512], start=(c == 0), stop=(c == NCH - 1)),
                                [Wo_ready, pA_free[i]] if c == 0 else [], sig=(c == NCH - 1))
                    ad = op("dve", lambda e, i=i, half=half: e.tensor_tensor(out=x1[b_][:, half * 512:(half + 1) * 512], in0=pA[i][:], in1=xr[b_][:, half * 512:(half + 1) * 512], op=ALU.add), [mm, ld, x1_free[b_]])
                    pA_free[i] = ad
                    hs.append(ad)
                xr_free[b_] = hs[1]
                x1w = dma("sp", osem[b_], X1_sc.ap()[r0:r0 + 128, :], x1[b_][:], hs)
                x1w_toks[b_] = x1w
                sq = op("act", lambda e: e.activation(out=junkf[b_][:], in_=x1[b_][:], func=AF.Square, accum_out=statf[b_][:, 0:1]), hs + [statf_free[b_]])
                r2 = rsqrt(statf[b_][:, 2:3], statf[b_][:, 0:1], D, [sq])
                hfo = op("dve", lambda e: e.scalar_tensor_tensor(out=hf2[b_][:], in0=x1[b_][:], scalar=statf[b_][:, 2:3], in1=gffn_b[:], op0=ALU.mult, op1=ALU.mult), [r2, hf_free[b_], gffn_ready])
                statf_free[b_] = hfo
                x1_free[b_] = x1w
                hbo = op("dve", lambda e: e.tensor_copy(out=hb_[b_][:], in_=hf2[b_][:]), [hfo, hbw[b_]])
                hbw[b_] = dma("sp", hbsem[b_], HB_sc.ap()[r0:r0 + 128, :], hb_[b_][:], [hbo])
                tile_ctx[n] = dict(hfo=hfo, hbo=hbo, r2=r2)

            def mid_(n):
                b_ = n % 2
                hfo = tile_ctx[n]['hfo']; hbo = tile_ctx[n]['hbo']; r2 = tile_ctx[n]['r2']
                tps = []
                tp2 = None
                for grp in range(2):
                    for c4 in range(4):
                        c = grp * 4 + c4
                        tp2 = op("pe", lambda e, c=c, c4=c4: e.transpose(tpF[:, c4 * 128:(c4 + 1) * 128], hf2[b_][:, c * 128:(c + 1) * 128], ident_f[:]), [hfo, dst["tpF_free"]] if c4 == 0 else [], sig=(c4 == 3))
                    cpf = op("act", lambda e, grp=grp: e.copy(out=hTf[:, grp * 4:(grp + 1) * 4, :], in_=tpF[:].rearrange("p (a t) -> p a t", t=128)), [tp2] + ([dst["hTf_free"]] if grp == 0 else []))
                    dst["tpF_free"] = cpf
                    tps.append(cpf)
                hf_free[b_] = tp2
                stat_prev = dst["rt_free"]
                lg = None
                for c in range(NCH):
                    lg = op("pe", lambda e, c=c: e.matmul(tpF[:, 0:36], lhsT=hTf[:, c, :], rhs=Wr[:, c, :], start=(c == 0), stop=(c == NCH - 1)), [tps[1], Wr_ready] if c == 0 else [], sig=(c == NCH - 1))
                dst["hTf_free"] = lg
                tile_ctx[n]['lg'] = lg; tile_ctx[n]['stat_prev'] = stat_prev

            def back(n):
                b_ = n % 2
                hfo = tile_ctx[n]['hfo']; hbo = tile_ctx[n]['hbo']; r2 = tile_ctx[n]['r2']; lg = tile_ctx[n]['lg']; stat_prev = tile_ctx[n]['stat_prev']
                q1 = op("dve", lambda e: e.tensor_tensor(out=rt[:, 0:36], in0=tpF[:, 0:36], in1=rbias_b[:], op=ALU.add), [lg, stat_prev, cdone, hbo])
                dst["tpF_free"] = q1
                q2 = op("dve", lambda e: e.tensor_reduce(out=stat[:, 4:5], in_=rt[:, 0:4], axis=AX.X, op=ALU.max), [q1, r2, hfo])
                q3 = op("dve", lambda e: e.tensor_scalar(out=rt[:, 40:44], in0=rt[:, 0:4], scalar1=stat[:, 4:5], scalar2=None, op0=ALU.is_ge), [q2])
                q4 = op("dve", lambda e: e.tensor_scalar(out=stat[:, 5:6], in0=stat[:, 4:5], scalar1=-1.0, scalar2=None, op0=ALU.mult), [q2])
                q5 = op("act", lambda e: e.activation(out=rt[:, 180:184], in_=rt[:, 0:4], func=AF.Exp, bias=stat[:, 5:6], accum_out=stat[:, 6:7]), [q4])
                q6 = op("dve", lambda e: e.reciprocal(out=stat[:, 7:8], in_=stat[:, 6:7]), [q5])
                q7 = op("dve", lambda e: e.tensor_scalar(out=rt[:, 44:48], in0=rt[:, 40:44], scalar1=1.0, scalar2=BIG, op0=ALU.subtract, op1=ALU.mult), [q3])
                q8 = op("dve", lambda e: e.tensor_tensor(out=rt[:, 48:80].rearrange("p (g j) -> p g j", j=8), in0=rt[:, 4:36].rearrange("p (g j) -> p g j", j=8),
                                                         in1=rt[:, 44:48].unsqueeze(2).to_broadcast([128, 4, 8]), op=ALU.add), [q7])
                q9 = op("dve", lambda e: e.tensor_reduce(out=stat[:, 8:9], in_=rt[:, 48:80], axis=AX.X, op=ALU.max), [q8])
                q10 = op("dve", lambda e: e.tensor_scalar(out=rt[:, 80:112], in0=rt[:, 48:80], scalar1=stat[:, 8:9], scalar2=None, op0=ALU.is_ge), [q9])
                q11 = op("dve", lambda e: e.scalar_tensor_tensor(out=rt[:, 184:216], in0=rt[:, 80:112], scalar=-BIG, in1=rt[:, 48:80], op0=ALU.mult, op1=ALU.add), [q10])
                q12 = op("dve", lambda e: e.tensor_reduce(out=stat[:, 9:10], in_=rt[:, 184:216], axis=AX.X, op=ALU.max), [q11])
                q13 = op("dve", lambda e: e.tensor_scalar(out=rt[:, 112:144], in0=rt[:, 48:80], scalar1=stat[:, 9:10], scalar2=None, op0=ALU.is_ge), [q12])
                q14 = op("dve", lambda e: e.tensor_scalar(out=stat[:, 10:11], in0=stat[:, 8:9], scalar1=-1.0, scalar2=None, op0=ALU.mult), [q9])
                q15 = op("act", lambda e: e.activation(out=rt[:, 144:176], in_=rt[:, 48:80], func=AF.Exp, bias=stat[:, 10:11]), [q14, q8])
                q16 = op("act", lambda e: e.activation(out=stat[:, 11:12], in_=stat[:, 9:10], func=AF.Exp, bias=stat[:, 10:11]), [q14, q12])
                q17 = op("dve", lambda e: e.tensor_scalar(out=stat[:, 12:13], in0=stat[:, 11:12], scalar1=1.0, scalar2=None, op0=ALU.add), [q16])
                q18 = op("dve", lambda e: e.reciprocal(out=stat[:, 13:14], in_=stat[:, 12:13]), [q17])
                q19 = op("dve", lambda e: e.tensor_tensor(out=stat[:, 14:15], in0=stat[:, 13:14], in1=stat[:, 7:8], op=ALU.mult), [q18, q6])
                q20 = op("dve", lambda e: e.scalar_tensor_tensor(out=G_all[:, n, :], in0=rt[:, 144:176], scalar=stat[:, 14:15], in1=rt[:, 112:144], op0=ALU.mult, op1=ALU.mult), [q19, q15, q13])
                q21 = op("dve", lambda e: e.tensor_copy(out=SELb[:, n, :], in_=rt[:, 112:144]), [q13])
                q22 = op("dve", lambda e: e.tensor_copy(out=SELf[:, n, :], in_=rt[:, 112:144]), [q13])
                q23 = op("dve", lambda e: e.tensor_copy(out=SEL1[:, n, :], in_=rt[:, 80:112]), [q10])
                dst["rt_free"] = q23
                dst["stat_free"] = q23

            front(0)
            for n in range(NOT_):
                if n + 1 < NOT_:
                    front(n + 1)
                mid_(n)
                back(n)
            router_done = dst["rt_free"]

            ptok = None
            for n in range(NOT_):
                col = (n % 8) * 32
                mm = None
                for n2 in range(n):
                    mm = op("pe", lambda e, n2=n2, col=col: e.matmul(pR[:, col:col + 32], lhsT=ones_bf[:], rhs=SELb[:, n2, :], start=(n2 == 0), stop=False, skip_group_check=True),
                            [router_done, cD, ptok] if n2 == 0 else [], sig=False)
                mm = op("pe", lambda e, n=n, col=col: e.matmul(pR[:, col:col + 32], lhsT=strict_s[:], rhs=SELb[:, n, :], start=(n == 0), stop=True, skip_group_check=True), [router_done, cD, ptok])
                ptok = op("dve", lambda e, n=n, col=col: e.tensor_copy(out=POS[:, n, :], in_=pR[:, col:col + 32]), [mm])
            mm = None
            for n2 in range(NOT_):
                mm = op("pe", lambda e, n2=n2: e.matmul(tpF[:, 64:96], lhsT=ones_bf[:], rhs=SELb[:, n2, :], start=(n2 == 0), stop=(n2 == NOT_ - 1), skip_group_check=True), [router_done] if n2 == 0 else [], sig=(n2 == NOT_ - 1))
            w1_ = op("dve", lambda e: e.tensor_copy(out=rt[:, 0:32], in_=tpF[:, 64:96]), [mm, ptok])
            NJ = 2 * NOWN // MOE_B
            w2_ = op("dve", lambda e: e.tensor_tensor(out=big[:, 0:32 * NJ].rearrange("p (e j) -> p e j", j=NJ), in0=rt[:, 0:32].unsqueeze(2).to_broadcast([128, 32, NJ]),
                                                      in1=thr_s[:, 0:NJ].unsqueeze(1).to_broadcast([128, 32, NJ]), op=ALU.is_gt), [w1_, cD])
            w3_ = op("dve", lambda e: e.tensor_reduce(out=rt[:, 32:64], in_=big[:, 0:32 * NJ].rearrange("p (e j) -> p e j", j=NJ), axis=AX.X, op=ALU.add), [w2_])
            w5_ = op("dve", lambda e: e.tensor_scalar(out=rt[:, 64:96], in0=rt[:, 32:64], scalar1=float(MOE_B), scalar2=None, op0=ALU.mult), [w3_])
            prev = op("dve", lambda e: e.tensor_copy(out=rt[:, 96:128], in_=rt[:, 64:96]), [w5_])
            src, dstc = 96, 128
            k = 1
            while k < 32:
                a1 = op("dve", lambda e, src=src, dstc=dstc, k=k: e.tensor_copy(out=rt[:, dstc:dstc + k], in_=rt[:, src:src + k]), [prev])
                prev = op("dve", lambda e, src=src, dstc=dstc, k=k: e.tensor_tensor(out=rt[:, dstc + k:dstc + 32], in0=rt[:, src + k:src + 32], in1=rt[:, src:src + 32 - k], op=ALU.add), [a1])
                src, dstc = dstc, src
                k *= 2
            pe_ = op("dve", lambda e, src=src: e.tensor_copy(out=rt[:, 160:192], in_=rt[:, src:src + 32]), [prev])
            ps_ = op("dve", lambda e: e.tensor_tensor(out=rt[:, 192:224], in0=rt[:, 160:192], in1=rt[:, 64:96], op=ALU.subtract), [pe_])
            dsum = sbA("dsum", [128, NOT_, 4], F32)
            bigv = big[:, 0:NOT_ * 32].rearrange("p (n e) -> p n e", e=32)
            v1 = op("dve", lambda e: e.tensor_tensor(out=POS[:], in0=POS[:], in1=rt[:, 192:224].unsqueeze(1).to_broadcast([128, NOT_, 32]), op=ALU.add), [ps_])
            v2 = op("dve", lambda e: e.tensor_tensor(out=bigv, in0=POS[:], in1=SEL1[:], op=ALU.mult), [v1])
            v3 = op("dve", lambda e: e.tensor_reduce(out=dsum[:, :, 0:1], in_=bigv, axis=AX.X, op=ALU.add), [v2])
            v4 = op("dve", lambda e: e.tensor_tensor(out=bigv, in0=POS[:], in1=SELf[:], op=ALU.mult), [v3])
            v5 = op("dve", lambda e: e.tensor_reduce(out=dsum[:, :, 1:2], in_=bigv, axis=AX.X, op=ALU.add), [v4])
            v6 = op("dve", lambda e: e.tensor_tensor(out=dsum[:, :, 1:2], in0=dsum[:, :, 1:2], in1=dsum[:, :, 0:1], op=ALU.subtract), [v5])
            v7 = op("dve", lambda e: e.tensor_copy(out=D12i[:], in_=dsum[:, :, 0:2]), [v6])
            v8 = op("dve", lambda e: e.tensor_tensor(out=bigv, in0=G_all[:], in1=SEL1[:], op=ALU.mult), [v7])
            v9 = op("dve", lambda e: e.tensor_reduce(out=GT[:, :, 0:1], in_=bigv, axis=AX.X, op=ALU.add), [v8])
            v10 = op("dve", lambda e: e.tensor_reduce(out=GT[:, :, 1:2], in_=G_all[:], axis=AX.X, op=ALU.add), [v9])
            v11 = op("dve", lambda e: e.tensor_tensor(out=GT[:, :, 1:2], in0=GT[:, :, 1:2], in1=GT[:, :, 0:1], op=ALU.subtract), [v10])
            dest_ready = v11
            bigb = big[:, 0:NB * 32].rearrange("p (b e) -> p b e", e=32)
            bex = sbA("bex", [128, NB], F32)
            idxf = sbA("idxf", [128, NB], F32)
            u1 = op("dve", lambda e: e.tensor_tensor(out=bigb, in0=rt[:, 160:192].unsqueeze(1).to_broadcast([128, NB, 32]), in1=thr_s[:].unsqueeze(2).to_broadcast([128, NB, 32]), op=ALU.is_le), [v11, cD])
            u2 = op("dve", lambda e: e.tensor_reduce(out=bex[:], in_=bigb, axis=AX.X, op=ALU.add), [u1])
            u3 = op("dve", lambda e: e.tensor_scalar(out=bex[:], in0=bex[:], scalar1=float(NEXP - 1), scalar2=None, op0=ALU.min), [u2])
            u4 = op("dve", lambda e: e.tensor_scalar(out=idxf[:], in0=bex[:], scalar1=128.0, scalar2=base_s[:, 0:1], op0=ALU.mult, op1=ALU.add), [u3])
            u5 = op("dve", lambda e: e.tensor_scalar(out=bex[:], in0=thr_s[:], scalar1=rt[:, 191:192], scalar2=None, op0=ALU.is_ge), [u4])
            u6 = op("dve", lambda e: e.scalar_tensor_tensor(out=idxf[:], in0=bex[:], scalar=float(NEXP * 128), in1=idxf[:], op0=ALU.mult, op1=ALU.add), [u5])
            u7 = op("dve", lambda e: e.tensor_copy(out=idxwi[:], in_=idxf[:]), [u6])
            idx_ready = u7

            scs = [kb.dsem(f"scat{i}") for i in range(NHB)]
            lss = [kb.dsem(f"hbl{i}") for i in range(NHB)]

            def hb_load(n):
                b_ = n % NHB
                dma("sp", lss[b_], hbl[b_][:], HB_sc.ap()[n * 128:(n + 1) * 128, :], [(scs[b_], scs[b_].count)] + hbw)

            for n in range(min(NHB, NOT_)):
                hb_load(n)
            kb.wait("pool", [dest_ready, ztok])
            for n in range(NOT_):
                b_ = n % NHB
                kb.wait("pool", [(lss[b_], lss[b_].count)])
                for k2 in range(2):
                    ins = nc.gpsimd.indirect_dma_start(out=XS_sc.ap(), out_offset=IOA(ap=D12i[:, n, k2:k2 + 1], axis=0), in_=hbl[b_][:], in_offset=None)
                    scs[b_].count += 16
                    ins.then_inc(scs[b_].h, 16)
                if n - 4 >= 0 and n - 4 + NHB < NOT_:
                    hb_load(n - 4 + NHB)
            kb.wait("pool", [(s_, s_.count) for s_ in scs])
            scat_done = op("pool", lambda e: e.memset(hbl[0][0:1, 0:2], 0.0))
            kb.wait("sp", x1w_toks + [scat_done])
            kb.drain_all()
        mid.close()
        nc.all_engine_barrier()

        with ExitStack() as ph:
            sbA = lambda name, shape, dt: ph.enter_context(nc.sbuf_tensor(name, list(shape), dt))
            psA = lambda name, shape, dt: ph.enter_context(nc.psum_tensor(name, list(shape), dt))
            W1 = [sbA(f"W1_{i}", [128, NCH, HID], BF16) for i in range(2)]
            W3 = [sbA(f"W3_{i}", [128, NCH, HID], BF16) for i in range(2)]
            W2 = [sbA(f"W2_{i}", [128, 4, D], BF16) for i in range(2)]
            Wst1 = [sbA(f"Wst1_{i}", [128, NCH, HID], F32) for i in range(2)]
            Wst3 = [sbA(f"Wst3_{i}", [128, NCH, HID], F32) for i in range(2)]
            Wst2 = [sbA(f"Wst2_{i}", [128, 4, D], F32) for i in range(2)]
            xblk = [sbA(f"xblk{i}", [128, 4, D], BF16) for i in range(2)]
            xT = [sbA(f"xTm{i}", [128, NCH, 512], BF16) for i in range(2)]
            sil = [sbA(f"sil{i}", [128, 512], BF16) for i in range(2)]
            actT = [sbA(f"actT{i}", [128, 4, 512], BF16) for i in range(2)]
            ysb1 = sbA("ysb", [128, 4, D], F32)
            ysb = [ysb1, ysb1]
            pA = [psA(f"pE{i}", [128, 512], F32) for i in range(4)]
            pY = [psA(f"pY{i}", [128, 512], F32) for i in range(2)]
            tpX = [psA(f"tpX{i}", [128, NCH, 128], BF16) for i in range(2)]
            pA_free = [None] * 4; pY_free = [None, None]; tpX_free = [None, None]
            xls = [kb.dsem("xsL0"), kb.dsem("xsL1")]
            yws = [kb.dsem("ysW0"), kb.dsem("ysW1")]
            w_free = [None, None]; xblk_free = [None, None]; xT_free = [None, None]; sil_free = [None, None]; act_free = [None, None]
            ysb_free = [None, None]
            yw_toks = [None, None]
            w1v = w1.ap().rearrange("e (p c) n -> (e p) (c n)", c=NCH)
            w3v = w3.ap().rearrange("e (p c) n -> (e p) (c n)", c=NCH)
            w2v = w2.ap().rearrange("e (p c) n -> (e p) (c n)", c=4)
            ntp = 0
            gsem2 = [kb.dsem("moeWst0"), kb.dsem("moeWst1")]
            bc_reg = nc.gpsimd.to_reg(NEXP * 128 - 1)
            st_free = [None, None]
            conv = {}
            gtoks = {}

            def gather_w(blk):
                sb_ = blk % 2
                kb.wait("pool", [st_free[sb_], idx_ready])
                for (dst_, src_) in ((Wst1[sb_], w1v), (Wst3[sb_], w3v), (Wst2[sb_], w2v)):
                    ins = nc.gpsimd.indirect_dma_start(out=dst_[:].rearrange("p c n -> p (c n)"), out_offset=None, in_=src_, in_offset=IOA(ap=idxwi[:, blk:blk + 1], axis=0),
                                                       bounds_check=bc_reg, oob_is_err=False)
                    gsem2[sb_].count += 16; ins.then_inc(gsem2[sb_].h, 16)
                gtoks[blk] = (gsem2[sb_], gsem2[sb_].count)

            def convert_w(blk):
                wb_ = blk % 2
                gtok = gtoks[blk]
                c1 = op("act", lambda e: e.copy(out=W1[wb_][:], in_=Wst1[wb_][:]), [gtok, w_free[wb_]])
                c2 = op("dve", lambda e: e.tensor_copy(out=W3[wb_][:], in_=Wst3[wb_][:]), [gtok, w_free[wb_]])
                c3 = op("act", lambda e: e.copy(out=W2[wb_][:, 0:2, :], in_=Wst2[wb_][:, 0:2, :]), [gtok])
                c4 = op("dve", lambda e: e.tensor_copy(out=W2[wb_][:, 2:4, :], in_=Wst2[wb_][:, 2:4, :]), [gtok])
                conv[blk] = [c3, c4]
                st_free[wb_] = None
                kb.wait("pool", [c3, c4])
                if blk + 2 < NB:
                    gather_w(blk + 2)

            gather_w(0)
            if NB > 1:
                gather_w(1)
            convert_w(0)
            for blk in range(NB):
                wb = blk % 2
                wready = None
                if blk == 0:
                    xl_next = dma("sp", xls[0], xblk[0][:], XS_sc.ap()[0:MOE_B, :].rearrange("(t p) d -> p t d", p=128), [scat_done])
                xl = xl_next
                if blk + 1 < NB:
                    xl_next = dma("sp", xls[1 - wb], xblk[1 - wb][:], XS_sc.ap()[(blk + 1) * MOE_B:(blk + 2) * MOE_B, :].rearrange("(t p) d -> p t d", p=128), [xblk_free[1 - wb], scat_done])
                cpx = None
                for tt in range(4):
                    tb = ntp % 2; ntp += 1
                    tp = None
                    for c in range(NCH):
                        tp = op("pe", lambda e, c=c, tt=tt, tb=tb: e.transpose(tpX[tb][:, c, :], xblk[wb][:, tt, c:D:NCH], ident_bf[:]), [xl, tpX_free[tb]] if c == 0 else [], sig=(c == NCH - 1))
                    cpx = op("act" if tt % 2 == 0 else "dve",
                             (lambda e, tt=tt, tb=tb: e.copy(out=xT[wb][:, :, tt * 128:(tt + 1) * 128], in_=tpX[tb][:])) if tt % 2 == 0 else
                             (lambda e, tt=tt, tb=tb: e.tensor_copy(out=xT[wb][:, :, tt * 128:(tt + 1) * 128], in_=tpX[tb][:])),
                             [tp] + ([xT_free[wb]] if tt < 2 else []))
                    tpX_free[tb] = cpx
                    xT_ready_prev = cpx
                xblk_free[wb] = (kb.esem["pe"], kb.esem["pe"].count)
                xT_ready = [(kb.esem["act"], kb.esem["act"].count), (kb.esem["dve"], kb.esem["dve"].count)]
                ab = blk % 2
                s2 = None
                for hc in range(4):
                    i1 = (2 * hc) % 4; i3 = (2 * hc + 1) % 4
                    m1 = None
                    for c in range(NCH):
                        m1 = op("pe", lambda e, c=c, hc=hc, i1=i1: e.matmul(pA[i1][:], lhsT=W1[wb][:, c, hc:HID:4], rhs=xT[wb][:, c, :], start=(c == 0), stop=(c == NCH - 1)),
                                (conv[blk] + [pA_free[i1]] + xT_ready) if c == 0 else [], sig=(c == NCH - 1))
                    m3 = None
                    for c in range(NCH):
                        m3 = op("pe", lambda e, c=c, hc=hc, i3=i3: e.matmul(pA[i3][:], lhsT=W3[wb][:, c, hc:HID:4], rhs=xT[wb][:, c, :], start=(c == 0), stop=(c == NCH - 1)),
                                [pA_free[i3]] if c == 0 else [], sig=(c == NCH - 1))
                    sb_i = hc % 2
                    s1 = op("act", lambda e, i1=i1, sb_i=sb_i: e.activation(out=sil[sb_i][:], in_=pA[i1][:], func=AF.Silu), [m1, sil_free[sb_i]])
                    pA_free[i1] = s1
                    s2 = op("dve", lambda e, i3=i3, sb_i=sb_i, hc=hc, ab=ab: e.tensor_tensor(out=actT[ab][:, hc, :], in0=pA[i3][:], in1=sil[sb_i][:], op=ALU.mult), [m3, s1] + ([act_free[ab]] if hc == 0 else []))
                    pA_free[i3] = s2
                    sil_free[sb_i] = s2
                xT_free[wb] = (kb.esem["pe"], kb.esem["pe"].count)
                a_ready = s2
                if blk + 1 < NB:
                    convert_w(blk + 1)
                lastpe = None
                ycp = None
                for tt in range(4):
                    for half in range(2):
                        yb = (tt * 2 + half) % 2
                        my = None
                        for hc in range(4):
                            my = op("pe", lambda e, hc=hc, tt=tt, half=half, yb=yb, ab=ab: e.matmul(pY[yb][:], lhsT=actT[ab][:, hc, tt * 128:(tt + 1) * 128], rhs=W2[wb][:, hc, half * 512:(half + 1) * 512], start=(hc == 0), stop=(hc == 3)),
                                    [a_ready, pY_free[yb]] if hc == 0 else [], sig=(hc == 3))
                        if half == 0:
                            ycp = op("act", lambda e, yb=yb, tt=tt, half=half: e.copy(out=ysb[wb][:, tt, half * 512:(half + 1) * 512], in_=pY[yb][:]), [my] + ([ysb_free[wb]] if tt == 0 else []))
                        else:
                            ycp = op("dve", lambda e, yb=yb, tt=tt, half=half: e.tensor_copy(out=ysb[wb][:, tt, half * 512:(half + 1) * 512], in_=pY[yb][:]), [my] + ([ysb_free[wb]] if tt == 0 else []))
                        pY_free[yb] = ycp
                        lastpe = my
                act_free[ab] = lastpe
                w_free[wb] = lastpe
                yw = dma("sp", yws[wb], YS_sc.ap()[blk * MOE_B:(blk + 1) * MOE_B, :].rearrange("(t p) d -> p t d", p=128), ysb[wb][:],
                         [(kb.esem["act"], kb.esem["act"].count), (kb.esem["dve"], kb.esem["dve"].count)])
                ysb_free[0] = yw; ysb_free[1] = yw
                yw_toks[wb] = yw
            kb.wait("sp", yw_toks)
            kb.wait("pool", yw_toks)
            kb.drain_all()
        nc.all_engine_barrier()

        with ExitStack() as ph:
            sbA = lambda name, shape, dt: ph.enter_context(nc.sbuf_tensor(name, list(shape), dt))
            xo = [sbA(f"xo{i}", [128, D], F32) for i in range(4)]
            y1 = [sbA(f"y1_{i}", [128, D], F32) for i in range(4)]
            y2 = [sbA(f"y2_{i}", [128, D], F32) for i in range(4)]
            xls = [kb.dsem(f"x1L{i}") for i in range(4)]
            gs_ = [kb.dsem(f"yG{i}") for i in range(4)]
            for s_ in gs_:
                kb.dma_sems.remove(s_)
            ows = [kb.dsem(f"oW{i}") for i in range(4)]
            xo_free = [None] * 4; y_free = [None] * 4
            ow = [None] * 4
            for n in range(NOT_):
                b_ = n % 4
                r0 = n * 128
                ld = dma("sp", xls[b_], xo[b_][:], X1_sc.ap()[r0:r0 + 128, :], [xo_free[b_]])
                kb.wait("pool", [y_free[b_]])
                ins = nc.gpsimd.indirect_dma_start(out=y1[b_][:], out_offset=None, in_=YS_sc.ap(), in_offset=IOA(ap=D12i[:, n, 0:1], axis=0))
                gs_[b_].count += 16; ins.then_inc(gs_[b_].h, 16)
                ins = nc.gpsimd.indirect_dma_start(out=y2[b_][:], out_offset=None, in_=YS_sc.ap(), in_offset=IOA(ap=D12i[:, n, 1:2], axis=0))
                gs_[b_].count += 16; ins.then_inc(gs_[b_].h, 16)
                gt = (gs_[b_], gs_[b_].count)
                c1 = op("dve", lambda e: e.scalar_tensor_tensor(out=xo[b_][:], in0=y1[b_][:], scalar=GT[:, n, 0:1], in1=xo[b_][:], op0=ALU.mult, op1=ALU.add), [ld, gt])
                c2 = op("dve", lambda e: e.scalar_tensor_tensor(out=xo[b_][:], in0=y2[b_][:], scalar=GT[:, n, 1:2], in1=xo[b_][:], op0=ALU.mult, op1=ALU.add), [c1])
                y_free[b_] = c2
                ow[b_] = dma("sp", ows[b_], out.ap()[r0:r0 + 128, :], xo[b_][:], [c2])
                xo_free[b_] = ow[b_]
            kb.wait("sp", ow)
            kb.dma_sems.extend(gs_)
            kb.drain_all()
    return nc


_CACHE = {}


def kernel(**inputs):
    x = np.asarray(inputs["x"], dtype=np.float32)
    B, S, _ = x.shape
    ncores = 2 * B
    if S not in _CACHE:
        _CACHE[S] = build_nc(S)
    nc = _CACHE[S]
    NSLOT = S // 1024
    consts = [host_constants(hf, (S + NEXP * MOE_B) // MOE_B) for hf in range(2)]
    in_maps = []
    own_idx = []
    for c in range(ncores):
        b, hf = c // 2, c % 2
        idx = np.concatenate([np.arange(512 * (2 * i + hf), 512 * (2 * i + hf) + 512) for i in range(NSLOT)])
        own_idx.append(idx)
        m = {"xb": np.ascontiguousarray(x[b]), "xq": np.ascontiguousarray(x[b][idx])}
        for k in ("g_attn", "w_in", "qn_g", "kn_g", "lam_q1", "lam_k1", "lam_q2", "lam_k2", "subln_g", "sb_out_g",
                  "w_o", "g_ffn", "w_router_g", "b_router_g", "w_router_e", "b_router_e", "w1", "w3", "w2"):
            m[k] = np.ascontiguousarray(np.asarray(inputs[k], dtype=np.float32)[0])
        m["rel_bias"] = np.ascontiguousarray(np.asarray(inputs["rel_bias"], dtype=np.float32))
        m.update(consts[hf])
        in_maps.append(m)
    res = run_bass_kernel_spmd(nc, in_maps, core_ids=list(range(ncores)))
    outp = np.empty((B, S, D), np.float32)
    for c in range(ncores):
        b = c // 2
        outp[b, own_idx[c]] = np.asarray(res.results[c]["out"], dtype=np.float32)
    return outp
```

```python
import math
import numpy as np
import ml_dtypes
import concourse.bass as bass
import concourse.mybir as mybir
from concourse.bass_utils import run_bass_kernel_spmd

F32 = mybir.dt.float32
BF16 = mybir.dt.bfloat16
AF = mybir.ActivationFunctionType
ALU = mybir.AluOpType
AX = mybir.AxisListType

D = 1024
NCH = 8
IN_W = 3072
NEXP = 32
HID = 512
EPS = 1e-6
LAMBDA_INIT = 0.8 - 0.6 * math.exp(-0.3 * 0)
NEG = -30000.0
BIG = 1.0e4
SB_DUMMY_A = 0
SB_DUMMY_B = 1
MOE_B = 512


class Sem:
    def __init__(self, handle):
        self.h = handle
        self.count = 0


class KB:
    def __init__(self, nc, stack):
        self.nc = nc
        self.stack = stack
        self.eng = {"pe": nc.tensor, "act": nc.scalar, "dve": nc.vector, "pool": nc.gpsimd, "sp": nc.sync}
        self.esem = {k: self.new_sem("e_" + k) for k in self.eng}
        self.waited = {k: {} for k in self.eng}
        self.dma_sems = []
        self.nsem = 0

    def new_sem(self, name):
        return Sem(self.stack.enter_context(self.nc.semaphore(name)))

    def dsem(self, name):
        s = self.new_sem("d_" + name)
        self.dma_sems.append(s)
        return s

    def wait(self, e, deps):
        w = self.waited[e]
        for d in deps:
            if d is None:
                continue
            s, v = d
            if w.get(id(s), 0) < v:
                self.eng[e].wait_ge(s.h, v)
                w[id(s)] = v

    def op(self, e, fn, deps=(), sig=True):
        self.wait(e, deps)
        ins = fn(self.eng[e])
        if sig:
            s = self.esem[e]
            s.count += 1
            ins.then_inc(s.h, 1)
            return (s, s.count)
        return None

    def dma(self, q, sem, out, in_, deps=()):
        self.wait(q, deps)
        ins = self.eng[q].dma_start(out=out, in_=in_)
        sem.count += 16
        ins.then_inc(sem.h, 16)
        return (sem, sem.count)

    def drain_all(self):
        toks = [(s, s.count) for s in self.esem.values() if s.count] + [(s, s.count) for s in self.dma_sems if s.count]
        for e in self.eng:
            self.wait(e, toks)


def T(ap_or_handle):
    return ap_or_handle


def rel_bucket_np(rel):
    nb = 16
    max_exact = 8
    base = np.where(rel > 0, nb, 0)
    n = np.abs(rel)
    nf = np.maximum(n, 1).astype(np.float32)
    large = max_exact + (np.log(nf / np.float32(max_exact)) / np.float32(math.log(128 / max_exact))
                         * np.float32(nb - max_exact)).astype(np.int32)
    large = np.minimum(large, nb - 1)
    return base + np.where(n < max_exact, n, large)


def host_constants(hf, nb=48):
    c = {}
    eye = np.eye(128, dtype=np.float32)
    c["ident_bf"] = eye.astype(ml_dtypes.bfloat16)
    c["ident_f"] = eye
    c["jrev_bf"] = eye[::-1].copy().astype(ml_dtypes.bfloat16)
    j = np.arange(128)
    c["negT_bf"] = (-(j[:, None] >= j[None, :]).astype(np.float32)).astype(ml_dtypes.bfloat16)
    c["ones_bf"] = np.ones((128, 128), np.float32).astype(ml_dtypes.bfloat16)
    blk = np.zeros((128, 128), np.float32)
    blk[:64, :64] = 1.0
    blk[64:, 64:] = 1.0
    c["blk_ones_f"] = blk
    kk = np.arange(128)[:, None]
    qcol = np.arange(512)[None, :]
    sbm = np.zeros((8, 128, 512), np.float32)
    for s in range(8):
        kpos = 128 * s + kk
        qpos = 128 * 4 * hf + qcol
        sbm[s] = np.where(kpos < qpos, 0.0, NEG)
    c["sbmask"] = sbm.astype(ml_dtypes.bfloat16)
    dam = np.zeros((9, 128, 512), np.float32)
    for si in range(9):
        s = si - 1
        kpos = 128 * s + kk + 1024
        qpos = 128 * 4 * hf + qcol + 1024
        vis = (kpos // 64) <= (qpos // 64)
        dam[si] = np.where(vis, 0.0, NEG)[::-1]
    c["damask"] = dam
    R0 = 1023 - 512 * hf
    m = np.arange(1664)
    bk = rel_bucket_np((R0 - m).astype(np.int32))
    oh = np.zeros((32, 1664), np.float32)
    oh[bk, m] = 1.0
    oh[15, :] -= 1.0
    c["ohd"] = oh
    pp = np.arange(128, dtype=np.float32)[:, None]
    c["thr_c"] = np.tile((MOE_B * np.arange(nb, dtype=np.float32))[None, :], (128, 1))
    c["base13"] = (pp + 128.0 * np.arange(8, dtype=np.float32)[None, :]).astype(np.float32)
    c["strict_bf"] = (j[:, None] < j[None, :]).astype(np.float32).astype(ml_dtypes.bfloat16)
    return c


def build_nc(S):
    from contextlib import ExitStack
    NSLOT = S // 1024
    NOWN = S // 2
    NKB = S // 128
    NST = S // 512
    NOST = NOWN // 512
    NOT_ = NOWN // 128
    CAP = 2 * NOWN + NEXP * MOE_B
    NB = CAP // MOE_B
    nc = bass.Bass("TRN2", target_bir_lowering=False)

    def din(name, shape, dt=F32):
        return nc.dram_tensor(name, list(shape), dt, kind="ExternalInput")

    xb = din("xb", [S, D])
    xq = din("xq", [NOWN, D])
    g_attn = din("g_attn", [D]); w_in = din("w_in", [D, IN_W])
    qn_g = din("qn_g", [64]); kn_g = din("kn_g", [64])
    lam_q1 = din("lam_q1", [64]); lam_k1 = din("lam_k1", [64]); lam_q2 = din("lam_q2", [64]); lam_k2 = din("lam_k2", [64])
    subln_g = din("subln_g", [128]); sb_out_g = din("sb_out_g", [64])
    rel_bias = din("rel_bias", [32, 4])
    w_o = din("w_o", [D, D]); g_ffn = din("g_ffn", [D])
    w_rg = din("w_router_g", [D, 4]); b_rg = din("b_router_g", [4])
    w_re = din("w_router_e", [D, 32]); b_re = din("b_router_e", [32])
    w1 = din("w1", [NEXP, D, HID]); w3 = din("w3", [NEXP, D, HID]); w2 = din("w2", [NEXP, HID, D])
    c_ident_bf = din("ident_bf", [128, 128], BF16); c_ident_f = din("ident_f", [128, 128])
    c_jrev = din("jrev_bf", [128, 128], BF16); c_negT = din("negT_bf", [128, 128], BF16)
    c_ones = din("ones_bf", [128, 128], BF16); c_blk = din("blk_ones_f", [128, 128])
    c_sbmask = din("sbmask", [8, 128, 512], BF16); c_damask = din("damask", [9, 128, 512])
    c_ohd = din("ohd", [32, 1664])
    c_thr = din("thr_c", [128, NB]); c_base13 = din("base13", [128, 8]); c_strict = din("strict_bf", [128, 128], BF16)
    out = nc.dram_tensor("out", [NOWN, D], F32, kind="ExternalOutput")

    KT_da = nc.dram_tensor("KT_da", [4, 128, S], BF16)
    QT_da = nc.dram_tensor("QT_da", [4, 128, NOWN], BF16)
    V_da = nc.dram_tensor("V_da", [S, 4, 129], BF16)
    KT_sb = nc.dram_tensor("KT_sb", [4, 128, S], BF16)
    QT_sb = nc.dram_tensor("QT_sb", [4, 128, NOWN], BF16)
    V_sb = nc.dram_tensor("V_sb", [S, 512], BF16)
    U_sc = nc.dram_tensor("U_sc", [4, 1664], F32)
    X1_sc = nc.dram_tensor("X1_sc", [NOWN, D], F32)
    XS_sc = nc.dram_tensor("XS_sc", [CAP, D], BF16)
    HB_sc = nc.dram_tensor("HB_sc", [NOWN, D], BF16)
    YS_sc = nc.dram_tensor("YS_sc", [CAP, D], F32)

    def bc_row(t, n, parts=128, off=0):
        return bass.AP(t, off, [[0, parts], [1, n]])

    with ExitStack() as top:
        top.enter_context(nc.allow_non_contiguous_dma(reason="small strided constant / router weight loads"))
        kb = KB(nc, top)
        op, dma = kb.op, kb.dma
        sb = lambda name, shape, dt: top.enter_context(nc.sbuf_tensor(name, list(shape), dt))

        def rsqrt(out_ap, in_ap, n, deps):
            a_ = op("act", lambda e: e.activation(out=out_ap, in_=in_ap, func=AF.Ln, scale=1.0 / n, bias=EPS), deps)
            return op("act", lambda e: e.activation(out=out_ap, in_=out_ap, func=AF.Exp, scale=-0.5), [a_])

        ident_bf = sb("ident_bf_s", [128, 128], BF16); ident_f = sb("ident_f_s", [128, 128], F32)
        jrev = sb("jrev_s", [128, 128], BF16); negT = sb("negT_s", [128, 128], BF16)
        ones_bf = sb("ones_s", [128, 128], BF16); blk_f = sb("blk_s", [128, 128], F32)
        gsub_b = sb("gsub_b", [128, 128], F32); gsb_c = sb("gsb_c", [128, 1], F32)
        lamv = sb("lamv", [128, 4, 64], F32); lamt = sb("lamt", [128, 8], F32); neglam = sb("neglam", [128, 1], F32)
        gatt_c = sb("gatt_c", [128, NCH], F32)
        rb_s = sb("rb_s", [32, 4], F32)
        rbias_b = sb("rbias_b", [128, 36], F32)
        D12i = sb("D12i", [128, NOT_, 2], mybir.dt.int32)
        GT = sb("GT", [128, NOT_, 2], F32)
        idxwi = sb("idxwi", [128, NB], mybir.dt.int32)

        cs = kb.dsem("const")
        toks = []
        for dst, src in ((ident_bf, c_ident_bf), (ident_f, c_ident_f), (jrev, c_jrev), (negT, c_negT),
                         (ones_bf, c_ones), (blk_f, c_blk), (rb_s, rel_bias)):
            toks.append(dma("sp", cs, dst[:], src.ap()))
        for r in range(8):
            pass
        toks.append(dma("sp", cs, gsub_b[:], bc_row(subln_g, 128)))
        toks.append(dma("sp", cs, gsb_c[0:64, :], sb_out_g.ap().rearrange("(p o) -> p o", o=1)))
        toks.append(dma("sp", cs, gsb_c[64:128, :], sb_out_g.ap().rearrange("(p o) -> p o", o=1)))
        for i_, t_ in enumerate((lam_q1, lam_k1, lam_q2, lam_k2)):
            toks.append(dma("sp", cs, lamv[:, i_, :], bc_row(t_, 64)))
        toks.append(dma("sp", cs, gatt_c[:], g_attn.ap().rearrange("(c p) -> p c", p=128)))
        toks.append(dma("sp", cs, rbias_b[:, 0:4], bc_row(b_rg, 4)))
        toks.append(dma("sp", cs, rbias_b[:, 4:36], bc_row(b_re, 32)))
        cdone = toks[-1]

        t0 = op("dve", lambda e: e.tensor_scalar(out=gsub_b[:], in0=gsub_b[:], scalar1=1.0 - LAMBDA_INIT, scalar2=None, op0=ALU.mult), [cdone])
        t1 = op("dve", lambda e: e.tensor_tensor(out=lamv[:, 0, :], in0=lamv[:, 0, :], in1=lamv[:, 1, :], op=ALU.mult), [cdone])
        t2 = op("dve", lambda e: e.tensor_tensor(out=lamv[:, 2, :], in0=lamv[:, 2, :], in1=lamv[:, 3, :], op=ALU.mult), [cdone])
        t3 = op("dve", lambda e: e.tensor_reduce(out=lamt[:, 0:1], in_=lamv[:, 0, :], axis=AX.X, op=ALU.add), [t1])
        t4 = op("dve", lambda e: e.tensor_reduce(out=lamt[:, 1:2], in_=lamv[:, 2, :], axis=AX.X, op=ALU.add), [t2])
        t5 = op("act", lambda e: e.activation(out=lamt[:, 2:4], in_=lamt[:, 0:2], func=AF.Exp), [t3, t4])
        t6 = op("dve", lambda e: e.tensor_tensor(out=lamt[:, 4:5], in0=lamt[:, 3:4], in1=lamt[:, 2:3], op=ALU.subtract), [t5])
        t7 = op("dve", lambda e: e.tensor_scalar(out=neglam[:], in0=lamt[:, 4:5], scalar1=-LAMBDA_INIT, scalar2=None, op0=ALU.add), [t6])
        setup_done = t7

        mid = ExitStack()
        gk_b = mid.enter_context(nc.sbuf_tensor("gk_b", [128, 8, 64], F32))
        gq_b = mid.enter_context(nc.sbuf_tensor("gq_b", [128, 8, 64], F32))
        OT = mid.enter_context(nc.sbuf_tensor("OT", [128, NCH, NOWN], BF16))
        gsm = kb.dsem("gqk")
        gtk = None
        for r in range(8):
            gtk = dma("sp", gsm, gk_b[:, r, :], bc_row(kn_g, 64))
            gtk = dma("sp", gsm, gq_b[:, r, :], bc_row(qn_g, 64))
        t0q = op("dve", lambda e: e.tensor_scalar(out=gq_b[:], in0=gq_b[:], scalar1=0.125, scalar2=None, op0=ALU.mult), [gtk])
        with ExitStack() as ph:
            sbA = lambda name, shape, dt: ph.enter_context(nc.sbuf_tensor(name, list(shape), dt))
            psA = lambda name, shape, dt: ph.enter_context(nc.psum_tensor(name, list(shape), dt))
            W = sbA("W", [128, NCH, IN_W], BF16)
            tpA = [psA(f"tpA{i}", [128, NCH, 128], BF16) for i in range(2)]
            pj = [psA(f"pj{i}", [128, 512], F32) for i in range(4)]
            tpK = [psA(f"tpK{i}", [128, 4, 128], BF16) for i in range(2)]

            wst = [sbA(f"wst{i}", [128, 512], F32) for i in range(2)]
            wsem = [kb.dsem("w0"), kb.dsem("w1")]
            wfree = [None, None]
            Wr_tok = {}
            nw = 0
            for cb in (1, 2, 4, 5, 0, 3):
                wtok = None
                for c in range(NCH):
                    b_ = nw % 2; nw += 1
                    ld = dma("sp", wsem[b_], wst[b_][:], w_in.ap()[c * 128:(c + 1) * 128, cb * 512:(cb + 1) * 512], [wfree[b_]])
                    wtok = op("dve", lambda e, b_=b_, c=c, cb=cb: e.tensor_scalar(out=W[:, c, cb * 512:(cb + 1) * 512], in0=wst[b_][:], scalar1=gatt_c[:, c:c + 1], scalar2=None, op0=ALU.mult), [ld, cdone])
                    wfree[b_] = wtok
                Wr_tok[cb] = wtok
            xt = [sbA(f"xt{i}", [128, D], F32) for i in range(2)]
            junk2 = [sbA(f"junkA{i}", [128, D], BF16) for i in range(2)]
            stat2 = [sbA(f"statA{i}", [128, 8], F32) for i in range(2)]
            xn = [sbA(f"xn{i}", [128, D], BF16) for i in range(2)]
            xnT = [sbA(f"xnT{i}", [128, NCH, 512], BF16) for i in range(2)]
            ksq = [sbA(f"ksq{i}", [128, 512], F32) for i in range(2)]
            kst8 = [sbA(f"kst8{i}", [128, 16], F32) for i in range(2)]
            kn1 = [sbA(f"kn1{i}", [128, 512], F32) for i in range(2)]
            kn2 = [sbA(f"kn2{i}", [128, 512], BF16) for i in range(2)]
            KTst = [sbA(f"KTst{i}", [128, 4, 512], BF16) for i in range(2)]
            Vst = [sbA(f"Vst{i}", [128, 4, 4, 129], BF16) for i in range(2)]
            KSst = [sbA(f"KSst{i}", [128, 4, 512], BF16) for i in range(2)]
            VSst = [sbA(f"VSst{i}", [128, 4, 512], BF16) for i in range(2)]
            vinit = [op("dve", lambda e, i=i: e.memset(Vst[i][:], 1.0)) for i in range(2)]

            xsem = [kb.dsem("x0"), kb.dsem("x1")]
            stsems = {(k_, i_): kb.dsem(f"st{k_}{i_}") for k_ in ("kt", "ks", "v", "vs") for i_ in range(2)}
            st = {"xt_free": [None, None], "xn_free": [None, None], "xnT_free": [[], []], "tpA_free": [None, None],
                  "pj_free": [None] * 4, "pji": 0, "tpK_free": [None, None], "tpKi": 0, "kn2_free": [None, None], "kn2i": 0,
                  "stg_free": {}, "tile": 0, "stat_free": [None, None], "ksq_free": [None, None], "kst_free": [None, None], "kn1_free": [None, None]}

            def next_pj():
                i = st["pji"]; st["pji"] = (i + 1) % 4
                return i

            def proj_token_major(xT, tt, col0, deps):
                i = next_pj()
                tok = None
                for c in range(NCH):
                    tok = op("pe", lambda e, c=c, i=i: e.matmul(pj[i][:], lhsT=xT[:, c, tt * 128:(tt + 1) * 128], rhs=W[:, c, col0:col0 + 512], start=(c == 0), stop=(c == NCH - 1)),
                             deps + [st["pj_free"][i], Wr_tok[col0 // 512]] if c == 0 else [], sig=(c == NCH - 1))
                return i, tok

            def proj_feat_major(xT, col0, deps):
                i = next_pj()
                tok = None
                for c in range(NCH):
                    tok = op("pe", lambda e, c=c, i=i: e.matmul(pj[i][:], lhsT=W[:, c, col0:col0 + 128], rhs=xT[:, c, :], start=(c == 0), stop=(c == NCH - 1)),
                             deps + [st["pj_free"][i], Wr_tok[col0 // 512]] if c == 0 else [], sig=(c == NCH - 1))
                return i, tok

            def norm_stages(src, T_, xT, xT_free):
                ctx = {}
                res = {}

                def mk_a(tt):
                    def a():
                        n = st["tile"]; st["tile"] += 1
                        b_ = n % 2
                        row0 = T_ * 512 + tt * 128
                        ld = dma("pool", xsem[b_], xt[b_][:], src.ap()[row0:row0 + 128, :], [st["xt_free"][b_]])
                        sq = op("act", lambda e: e.activation(out=junk2[b_][:], in_=xt[b_][:], func=AF.Square, accum_out=stat2[b_][:, 0:1]), [ld, st["stat_free"][b_]])
                        r2 = rsqrt(stat2[b_][:, 2:3], stat2[b_][:, 0:1], D, [sq])
                        xo = op("dve", lambda e: e.tensor_scalar(out=xn[b_][:], in0=xt[b_][:], scalar1=stat2[b_][:, 2:3], scalar2=None, op0=ALU.mult), [r2, st["xn_free"][b_]])
                        st["xt_free"][b_] = xo
                        st["stat_free"][b_] = xo
                        ctx[tt] = (b_, xo)
                    return a

                def mk_b(tt):
                    def b():
                        b_, xo = ctx[tt]
                        tp = None
                        for c in range(NCH):
                            tp = op("pe", lambda e, c=c: e.transpose(tpA[b_][:, c, :], xn[b_][:, c * 128:(c + 1) * 128], ident_bf[:]),
                                    [xo, st["tpA_free"][b_], cdone] if c == 0 else [], sig=(c == NCH - 1))
                        st["xn_free"][b_] = tp
                        cp = op("dve", lambda e: e.tensor_copy(out=xT[:, :, tt * 128:(tt + 1) * 128], in_=tpA[b_][:]), [tp] + (xT_free if tt == 0 else []))
                        st["tpA_free"][b_] = cp
                        res["last"] = cp
                    return b
                return [mk_a(t) for t in range(4)], [mk_b(t) for t in range(4)], res

            def da_qk(xT, xready, T_, col0, g_b, dstT, ib):
                stg = KTst[ib]
                fr = st["stg_free"].get(("kt", ib))
                cps = []
                chain = {}

                def proj_chain(tt):
                    i, mm = proj_token_major(xT, tt, col0, [xready])
                    j = tt % 2
                    s1 = op("act", lambda e: e.activation(out=ksq[j][:], in_=pj[i][:], func=AF.Square), [mm, st["ksq_free"][j]])
                    s2 = op("dve", lambda e: e.tensor_reduce(out=kst8[j][:, 0:8], in_=ksq[j][:].rearrange("p (g d) -> p g d", d=64), axis=AX.X, op=ALU.add), [s1, st["kst_free"][j]])
                    st["ksq_free"][j] = s2
                    s4 = rsqrt(kst8[j][:, 8:16], kst8[j][:, 0:8], 64, [s2])
                    s5 = op("dve", lambda e: e.tensor_tensor(out=kn1[j][:].rearrange("p (g d) -> p g d", d=64), in0=pj[i][:].rearrange("p (g d) -> p g d", d=64),
                                                           in1=kst8[j][:, 8:16].unsqueeze(2).to_broadcast([128, 8, 64]), op=ALU.mult), [s4, st["kn1_free"][j]])
                    st["pj_free"][i] = s5
                    st["kst_free"][j] = s5
                    s6 = op("dve", lambda e: e.tensor_tensor(out=kn2[j][:], in0=kn1[j][:], in1=g_b[:].rearrange("p g d -> p (g d)"), op=ALU.mult), [s5, st["kn2_free"][j], t0, t0q])
                    st["kn1_free"][j] = s6
                    chain[tt] = s6

                def transp(tt):
                    j = tt % 2
                    k_ = st["tpKi"]; st["tpKi"] = (k_ + 1) % 2
                    tp = None
                    for h in range(4):
                        tp = op("pe", lambda e, h=h: e.transpose(tpK[k_][:, h, :], kn2[j][:, h * 128:(h + 1) * 128], ident_bf[:]),
                                [chain[tt], st["tpK_free"][k_]] if h == 0 else [], sig=(h == 3))
                    st["kn2_free"][j] = tp
                    cp = op("act", lambda e: e.copy(out=stg[:, :, tt * 128:(tt + 1) * 128], in_=tpK[k_][:]), [tp] + ([fr] if tt == 0 else []))
                    st["tpK_free"][k_] = cp
                    cps.append(cp)

                proj_chain(0); proj_chain(1); transp(0); proj_chain(2); transp(1); proj_chain(3)

                def tail():
                    transp(2); transp(3)
                    w = dma("sp", stsems[("kt", ib)], dstT.ap()[:, :, T_ * 512:(T_ + 1) * 512].rearrange("h p t -> p h t"), stg[:], [cps[-1]])
                    st["stg_free"][("kt", ib)] = w
                return tail

            def sb_qk(xT, xready, T_, col0, dstT, ib, scale):
                stg = KSst[ib]
                fr = st["stg_free"].get(("ks", ib))
                cp = None
                for p_ in range(4):
                    i, mm = proj_feat_major(xT, col0 + p_ * 128, [xready])
                    cp = op("dve", lambda e, i=i, p_=p_: e.tensor_scalar(out=stg[:, p_, :], in0=pj[i][:], scalar1=scale, scalar2=None, op0=ALU.mult), [mm] + ([fr] if p_ == 0 else []))
                    st["pj_free"][i] = cp
                w = dma("sp", stsems[("ks", ib)], dstT.ap()[:, :, T_ * 512:(T_ + 1) * 512].rearrange("h p t -> p h t"), stg[:], [cp])
                st["stg_free"][("ks", ib)] = w

            def da_v(xT, xready, T_, ib):
                stg = Vst[ib]
                fr = st["stg_free"].get(("v", ib))
                cp = None
                for tt in range(4):
                    i, mm = proj_token_major(xT, tt, 1024, [xready])
                    cp = op("act", lambda e, i=i, tt=tt: e.copy(out=stg[:, tt, :, 0:128], in_=pj[i][:].rearrange("p (h d) -> p h d", d=128)), [mm, vinit[ib]] + ([fr] if tt == 0 else []))
                    st["pj_free"][i] = cp
                w = dma("sp", stsems[("v", ib)], V_da.ap()[T_ * 512:(T_ + 1) * 512, :, :].rearrange("(tt p) h e -> p tt h e", p=128), stg[:], [cp])
                st["stg_free"][("v", ib)] = w

            def sb_v(xT, xready, T_, ib):
                stg = VSst[ib]
                fr = st["stg_free"].get(("vs", ib))
                cp = None
                for tt in range(4):
                    i, mm = proj_token_major(xT, tt, 2560, [xready])
                    cp = op("dve", lambda e, i=i, tt=tt: e.tensor_copy(out=stg[:, tt, :], in_=pj[i][:]), [mm] + ([fr] if tt == 0 else []))
                    st["pj_free"][i] = cp
                w = dma("sp", stsems[("vs", ib)], V_sb.ap()[T_ * 512:(T_ + 1) * 512, :].rearrange("(tt p) f -> p tt f", p=128), stg[:], [cp])
                st["stg_free"][("vs", ib)] = w

            jobs = [("kv", T_) for T_ in range(NST)] + [("q", T_) for T_ in range(NOST)]

            def prep(j):
                kind, T_ = jobs[j]
                ib = j % 2
                return norm_stages(xb if kind == "kv" else xq, T_, xnT[ib], st["xnT_free"][ib])

            a_st, b_st, res = prep(0)
            for t in range(4):
                a_st[t](); b_st[t]()
            for j, (kind, T_) in enumerate(jobs):
                ib = j % 2
                xready = res["last"]
                if j + 1 < len(jobs):
                    na, nb_, nres = prep(j + 1)
                else:
                    na = nb_ = [lambda: None] * 4
                    nres = None
                if kind == "kv":
                    tail = da_qk(xnT[ib], xready, T_, 512, gk_b, KT_da, ib)
                    na[0]()
                    da_v(xnT[ib], xready, T_, ib)
                    nb_[0](); na[1]()
                    tail()
                    nb_[1](); na[2]()
                    sb_qk(xnT[ib], xready, T_, 2048, KT_sb, ib, 1.0)
                    nb_[2](); na[3]()
                    sb_v(xnT[ib], xready, T_, ib)
                    nb_[3]()
                else:
                    tail = da_qk(xnT[ib], xready, T_, 0, gq_b, QT_da, ib)
                    na[0](); na[1]()
                    sb_qk(xnT[ib], xready, T_, 1536, QT_sb, ib, 0.125)
                    nb_[0](); nb_[1](); na[2]()
                    tail()
                    nb_[2](); na[3](); nb_[3]()
                st["xnT_free"][ib] = [(kb.esem["pe"], kb.esem["pe"].count)]
                res = nres
            kb.drain_all()
        nc.all_engine_barrier()

        with ExitStack() as ph:
            sbA = lambda name, shape, dt: ph.enter_context(nc.sbuf_tensor(name, list(shape), dt))
            psA = lambda name, shape, dt: ph.enter_context(nc.psum_tensor(name, list(shape), dt))
            Trev = sbA("Trev", [128, 4, 9, 512], BF16)
            Pbw = [sbA(f"Pw{i}", [128, 1024], BF16) for i in range(3)]
            Pb = [[Pbw[i][:, m * 512:(m + 1) * 512] for i in range(3)] for m in range(2)]
            Ocp = sbA("Ocp", [128, 8, 129], F32)
            est = sbA("est", [128, 48], F32)
            eo1 = sbA("eo1", [128, 128], F32); ejk = sbA("ejk", [128, 128], F32)
            eo2 = [sbA(f"eo2_{i}", [128, 128], F32) for i in range(4)]
            eob = [sbA(f"eob{i}", [128, 128], BF16) for i in range(4)]
            Sb2 = [psA(f"S2_{i}", [128, 1024], F32) for i in range(2)]
            Sb = [[Sb2[i][:, m * 512:(m + 1) * 512] for i in range(2)] for m in range(2)]
            accO = [psA(f"accO{i}", [128, 512], F32) for i in range(3)]
            tpO = psA("tpO", [128, 8, 128], BF16)

            KT0 = sbA("KTd0", [128, S], BF16); QT0 = sbA("QTd0", [128, NOWN], BF16); Vd0 = sbA("Vd0", [128, NKB, 129], BF16)
            ldsem = [kb.dsem("daL0"), kb.dsem("daL1")]
            h0a = dma("sp", ldsem[0], KT0[:], KT_da.ap()[0])
            h0a = dma("sp", ldsem[0], QT0[:], QT_da.ap()[0])
            h0a = dma("sp", ldsem[0], Vd0[:], V_da.ap()[:, 0, :].rearrange("(k p) e -> p k e", p=128))
            tmpsc = ExitStack()
            sbT = lambda name, shape, dt: tmpsc.enter_context(nc.sbuf_tensor(name, list(shape), dt))
            ohd_s = sbT("ohd_s", [32, 1664], F32)
            u_s = sbT("u_s", [4, 1664], F32)
            NTB = 5
            tst = [sbT(f"tst{i}", [128, 512], F32) for i in range(NTB)]
            dmk9 = sbT("dmk9", [128, 9, 512], F32)
            bs = kb.dsem("bias")
            l0 = dma("sp", bs, ohd_s[:], c_ohd.ap())
            utok = None
            for q4 in range(4):
                mm = op("pe", lambda e, q4=q4: e.matmul(Sb[0][0][0:4, 0:416], lhsT=rb_s[:, :], rhs=ohd_s[:, q4 * 416:(q4 + 1) * 416], start=True, stop=True), [l0, cdone, utok])
                utok = op("dve", lambda e, q4=q4: e.tensor_copy(out=u_s[:, q4 * 416:(q4 + 1) * 416], in_=Sb[0][0][0:4, 0:416]), [mm])
            uw = dma("sp", bs, U_sc.ap(), u_s[:], [utok])
            kb.wait("sp", [uw])
            tfree = [None] * NTB
            bst = [kb.dsem(f"biasT{i}") for i in range(NTB)]
            dms = kb.dsem("dmk9")
            dml = dma("sp", dms, dmk9[:], c_damask.ap().rearrange("s p q -> p s q"))
            tlast = None
            n_ = 0
            for h in range(4):
                for si in range(9):
                    b_ = n_ % NTB; n_ += 1
                    off = 128 * (7 - (si - 1))
                    l1 = dma("sp", bst[b_], tst[b_][:], bass.AP(U_sc, h * 1664 + off, [[1, 128], [1, 512]]), [uw, tfree[b_]])
                    tlast = op("dve", lambda e, b_=b_, h=h, si=si: e.tensor_tensor(out=Trev[:, h, si, :], in0=tst[b_][:], in1=dmk9[:, si, :], op=ALU.add), [l1, dml])
                    tfree[b_] = tlast
            bias_ready = tlast
            tmpsc.close()
            KT = [KT0, sbA("KTd1", [128, S], BF16)]
            QT = [QT0, sbA("QTd1", [128, NOWN], BF16)]
            Vd = [Vd0, sbA("Vd1", [128, NKB, 129], BF16)]

            hfree = [None, bias_ready]
            acc_free = None
            tp_free = None
            S_free = [[utok, None], [None, None]]
            P_free = [[None] * 3, [None] * 3]
            un = 0
            eobi = 0
            ocp_free = None
            pending = []
            tpf = [None]; st2_prev = [None]; st3_prev = [None]

            def load_head(h):
                b_ = h % 2
                a = dma("sp", ldsem[b_], KT[b_][:], KT_da.ap()[h], [hfree[b_]])
                a = dma("sp", ldsem[b_], QT[b_][:], QT_da.ap()[h], [hfree[b_]])
                a = dma("sp", ldsem[b_], Vd[b_][:], V_da.ap()[:, h, :].rearrange("(k p) e -> p k e", p=128), [hfree[b_]])
                return a

            hl = {0: h0a}
            for h in range(4):
                b_ = h % 2
                if h + 1 < 4:
                    hl[h + 1] = load_head(h + 1)
                hready = hl[h]
                for i in range(NSLOT):
                    nkb = 8 * i + 8
                    q0 = i * 512
                    units = list(range(nkb))
                    stok = {}
                    ptok = {}
                    pvlast = None

                    def emit_qk(k_):
                        u_ = un + k_
                        sbk = u_ % 2
                        s_ = k_ - 8 * i
                        special = s_ >= -1
                        toks2 = []
                        for m in range(2):
                            r0 = 64 * m
                            tk = op("pe", lambda e, m=m, r0=r0, sbk=sbk: e.matmul(Sb[m][sbk][:], lhsT=KT[b_][r0:r0 + 64, k_ * 128:(k_ + 1) * 128], rhs=QT[b_][r0:r0 + 64, q0:q0 + 512], start=True, stop=not special),
                                    [hready, S_free[m][sbk], setup_done], sig=not special)
                            if special:
                                tk = op("pe", lambda e, m=m, sbk=sbk, s_=s_: e.matmul(Sb[m][sbk][:], lhsT=jrev[:], rhs=Trev[:, h, s_ + 1, :], start=False, stop=True), [bias_ready])
                            toks2.append(tk)
                        stok[k_] = toks2

                    def emit_exp(k_):
                        u_ = un + k_
                        sbk = u_ % 2
                        pb = u_ % 3
                        tk = op("act", lambda e: e.activation(out=Pbw[pb][:], in_=Sb2[sbk][:], func=AF.Exp), [stok[k_][0], stok[k_][1], P_free[0][pb], P_free[1][pb]])
                        S_free[0][sbk] = tk
                        S_free[1][sbk] = tk
                        ptok[k_] = [tk, tk]

                    def emit_pv(k_):
                        nonlocal pvlast
                        u_ = un + k_
                        pb = u_ % 3
                        for m in range(2):
                            tk = None
                            for qb in range(4):
                                a_ = m * 4 + qb
                                tk = op("pe", lambda e, m=m, qb=qb, a_=a_, pb=pb: e.matmul(accO[a_ // 3][:, (a_ % 3) * 129:(a_ % 3) * 129 + 129], lhsT=Pb[m][pb][:, qb * 128:(qb + 1) * 128], rhs=Vd[b_][:, k_, :], start=(k_ == 0 and a_ % 3 == 0), stop=(k_ == nkb - 1 and (a_ % 3 == 2 or a_ == 7)), skip_group_check=True),
                                        [ptok[k_][m]] + ([acc_free] if k_ == 0 else []), sig=(qb == 3))
                            P_free[m][pb] = tk
                            pvlast = tk

                    emit_qk(0)
                    for r in range(nkb + 1):
                        if r + 1 < nkb:
                            emit_qk(r + 1)
                        if r < nkb:
                            emit_exp(r)
                        if r - 1 >= 0:
                            emit_pv(r - 1)
                        while pending and pending[0][0] <= r:
                            pending.pop(0)[1]()
                    while pending:
                        pending.pop(0)[1]()
                    un += nkb
                    cpt = None
                    for bk in range(3):
                        na = 3 if bk < 2 else 2
                        cpt = op("dve", lambda e, bk=bk, na=na: e.tensor_copy(out=Ocp[:, bk * 3:bk * 3 + na, :], in_=accO[bk][:, 0:na * 129].rearrange("p (a e) -> p a e", e=129)), [pvlast, ocp_free])
                    acc_free = cpt
                    e5s = []
                    e7 = None
                    for qb in range(4):
                        c0 = qb * 8
                        e1 = op("dve", lambda e, qb=qb, c0=c0: e.reciprocal(out=est[:, c0:c0 + 1], in_=Ocp[:, qb, 128:129]), [cpt, st3_prev[0]])
                        e2 = op("dve", lambda e, qb=qb, c0=c0: e.reciprocal(out=est[:, c0 + 1:c0 + 2], in_=Ocp[:, 4 + qb, 128:129]), [cpt])
                        e3 = op("dve", lambda e, c0=c0: e.tensor_scalar(out=est[:, c0 + 2:c0 + 3], in0=est[:, c0 + 1:c0 + 2], scalar1=neglam[:, 0:1], scalar2=None, op0=ALU.mult), [e2, setup_done])
                        e4 = op("dve", lambda e, qb=qb, c0=c0: e.tensor_scalar(out=eo1[:], in0=Ocp[:, qb, 0:128], scalar1=est[:, c0:c0 + 1], scalar2=None, op0=ALU.mult), [e1, e7])
                        e5 = op("dve", lambda e, qb=qb, c0=c0: e.scalar_tensor_tensor(out=eo2[qb][:], in0=Ocp[:, 4 + qb, 0:128], scalar=est[:, c0 + 2:c0 + 3], in1=eo1[:], op0=ALU.mult, op1=ALU.add), [e3, e4, st2_prev[0]])
                        e6 = op("dve", lambda e, qb=qb: e.tensor_tensor(out=ejk[:], in0=eo2[qb][:], in1=eo2[qb][:], op=ALU.mult), [e5])
                        e7 = op("dve", lambda e, qb=qb: e.tensor_reduce(out=est[:, 32 + qb:33 + qb], in_=ejk[:], axis=AX.X, op=ALU.add), [e6])
                        e5s.append(e5)
                    ocp_free = e5s[-1]
                    ctx = {"e7": e7}

                    def stage1(ctx=ctx):
                        ctx["e9"] = rsqrt(est[:, 36:40], est[:, 32:36], 128, [ctx["e7"]])

                    def stage2(ctx=ctx):
                        tpl = None
                        ep = None
                        for qb in range(4):
                            ep = op("dve", lambda e, qb=qb: e.scalar_tensor_tensor(out=eob[qb][:], in0=eo2[qb][:], scalar=est[:, 36 + qb:37 + qb], in1=gsub_b[:], op0=ALU.mult, op1=ALU.mult), [ctx["e9"], t0, tpf[0]])
                            tpl = op("pe", lambda e, qb=qb: e.transpose(tpO[:, qb, :], eob[qb][:], ident_bf[:]), [ep, tpf[0]])
                        st2_prev[0] = ep
                        ctx["tpl"] = tpl

                    def stage3(ctx=ctx, h=h, q0=q0):
                        tpf[0] = op("act", lambda e: e.copy(out=OT[:, h, q0:q0 + 512], in_=tpO[:, 0:4, :].rearrange("p a t -> p (a t)")), [ctx["tpl"]])
                        st3_prev[0] = tpf[0]

                    pending.extend([(8, stage1), (10, stage2), (12, stage3)])
                hfree[b_] = (kb.esem["pe"], kb.esem["pe"].count)
            while pending:
                pending.pop(0)[1]()
            kb.drain_all()
        nc.all_engine_barrier()

        with ExitStack() as ph:
            sbA = lambda name, shape, dt: ph.enter_context(nc.sbuf_tensor(name, list(shape), dt))
            psA = lambda name, shape, dt: ph.enter_context(nc.psum_tensor(name, list(shape), dt))
            sbm = sbA("sbm", [128, 8, 512], BF16)
            KT = [sbA(f"KTs{i}", [128, S], BF16) for i in range(2)]
            QT = [sbA(f"QTs{i}", [128, NOWN], BF16) for i in range(2)]
            Vs1 = sbA("Vs", [128, NKB, 2, 128], BF16)
            Vs = [Vs1, Vs1]
            Eb = [sbA(f"Eb{i}", [128, 512], F32) for i in range(3)]
            Lb = [sbA(f"Lb{i}", [128, 512], BF16) for i in range(3)]
            Ab = [sbA(f"Ab{i}", [128, 512], F32) for i in range(3)]
            Pb2 = [sbA(f"Pa{i}", [128, 512], BF16) for i in range(3)]
            Rc = [sbA(f"Rc{i}", [128, 512], F32) for i in range(3)]
            sq_s = sbA("sq_s", [128, 512], F32)
            rs_s = sbA("rs_s", [128, 512], F32)
            Zb = [psA(f"Z{i}", [128, 512], F32) for i in range(4)]
            Racc = psA("Racc", [128, 512], F32)
            Oacc = [psA(f"Oacc{i}", [128, 512], F32) for i in range(2)]
            ssqb = psA("ssqb", [128, 512], F32)

            ztC = sbA("ztC", [128, 2048], BF16)
            zm = op("pool", lambda e: e.memset(ztC[:], 0.0))
            zsem = kb.dsem("zfill")
            zstate = {"next": 0, "tok": None}
            rows_per = 128 * 2

            def zero_fill_step(dep=None):
                r_ = zstate["next"]
                if r_ >= CAP:
                    return
                zstate["tok"] = dma("pool", zsem, XS_sc.ap()[r_:r_ + rows_per, :].rearrange("(p a) d -> p (a d)", p=128), ztC[:], [zm, dep])
                zstate["next"] = r_ + rows_per
            ms = kb.dsem("sbm")
            mload = dma("sp", ms, sbm[:], c_sbmask.ap().rearrange("s p q -> p s q"))
            vz0 = op("dve", lambda e: e.memset(Vs1[:], 0.0))
            vz = [vz0, vz0]
            vsem = kb.dsem("sbV")
            vfree = [None]
            vl = {}
            ldsem = [kb.dsem("sbL0"), kb.dsem("sbL1")]
            pfree = [None, None]

            def load_pair(p_):
                b_ = p_ % 2
                a = dma("sp", ldsem[b_], KT[b_][:], KT_sb.ap()[p_], [pfree[b_]])
                a = dma("sp", ldsem[b_], QT[b_][:], QT_sb.ap()[p_], [pfree[b_]])
                return a

            def load_v(p_):
                a = dma("sp", vsem, Vs1[:, :, 0, 0:64], V_sb.ap()[:, p_ * 128:p_ * 128 + 64].rearrange("(k p) e -> p k e", p=128), [vfree[0], vz0])
                a = dma("sp", vsem, Vs1[:, :, 1, 64:128], V_sb.ap()[:, p_ * 128 + 64:p_ * 128 + 128].rearrange("(k p) e -> p k e", p=128), [vfree[0], vz0])
                vl[p_] = a

            units = []
            for p_ in range(4):
                for i in range(NSLOT):
                    nkb = 8 * i + 8
                    for hh in range(2):
                        for k_ in range(nkb - 1, -1, -1):
                            units.append(dict(p=p_, i=i, hh=hh, k=k_, first=(k_ == nkb - 1), ofirst=(hh == 0 and k_ == nkb - 1), olast=(hh == 1 and k_ == 0)))
            NU = len(units)
            pl = {0: load_pair(0), 1: load_pair(1)}
            load_v(0)
            tk = {}
            Z_free = [None] * 4; E_free = [None] * 3; L_free = [[None, None] for _ in range(3)]; A_free = [None] * 3; P_free = [None] * 3
            O_free = [None, None]
            state = {"s7_prev": None, "s5_prev": None, "ssq_free": None, "sq_free": None, "rs_free": None, "ogrp": 0}

            def S1(u):
                U = units[u]; b_ = U["p"] % 2; z = u % 4; r0 = 64 * U["hh"]; k_ = U["k"]; q0 = U["i"] * 512
                s_ = k_ - 8 * U["i"]
                special = s_ >= 0
                t_ = op("pe", lambda e: e.matmul(Zb[z][:], lhsT=KT[b_][r0:r0 + 64, k_ * 128:(k_ + 1) * 128], rhs=QT[b_][r0:r0 + 64, q0:q0 + 512], start=True, stop=not special),
                        [pl[U["p"]], Z_free[z]], sig=not special)
                if special:
                    t_ = op("pe", lambda e: e.matmul(Zb[z][:], lhsT=ident_bf[:], rhs=sbm[:, s_, :], start=False, stop=True), [mload, cdone])
                tk[("s1", u)] = t_

            def S2(u):
                z = u % 3
                tk[("s2", u)] = op("act", lambda e: e.activation(out=Eb[z][:], in_=Zb[u % 4][:], func=AF.Exp), [tk[("s1", u)], E_free[z]])

            def S3(u):
                z = u % 3
                t_ = op("act", lambda e: e.activation(out=Lb[z][:], in_=Eb[z][:], func=AF.Ln, bias=1.0), [tk[("s2", u)]] + L_free[z])
                E_free[z] = t_
                tk[("s3", u)] = t_

            def S4(u):
                z = u % 3
                t_ = op("pe", lambda e: e.matmul(Zb[u % 4][:], lhsT=negT[:], rhs=Lb[z][:], start=False, stop=True, skip_group_check=True), [tk[("s3", u)], cdone])
                L_free[z][0] = t_
                tk[("s4", u)] = t_

            def S5a(u):
                z = u % 3
                U = units[u]
                if U["first"]:
                    tk[("s5a", u)] = None
                    return
                tk[("s5a", u)] = op("dve", lambda e: e.tensor_copy(out=Rc[z][:], in_=Racc[:]), [state["s7_prev"], tk.get(("s5", u - 3))])

            def S5(u):
                z = u % 3
                U = units[u]
                if U["first"]:
                    t_ = op("dve", lambda e: e.tensor_copy(out=Ab[z][:], in_=Zb[u % 4][:]), [tk[("s4", u)], A_free[z]])
                else:
                    t_ = op("dve", lambda e: e.tensor_tensor(out=Ab[z][:], in0=Zb[u % 4][:], in1=Rc[z][:], op=ALU.subtract), [tk[("s4", u)], A_free[z], tk[("s5a", u)]])
                Z_free[u % 4] = t_
                tk[("s5", u)] = t_

            def S7(u):
                z = u % 3
                U = units[u]
                t_ = op("pe", lambda e: e.matmul(Racc[:], lhsT=ones_bf[:], rhs=Lb[z][:], start=U["first"], stop=True, skip_group_check=True), [tk[("s5a", u)], tk[("s3", u)]])
                L_free[z][1] = t_
                state["s7_prev"] = t_
                tk[("s7", u)] = t_

            def S6(u):
                z = u % 3
                t_ = op("act", lambda e: e.activation(out=Pb2[z][:], in_=Ab[z][:], func=AF.Exp), [tk[("s5", u)], P_free[z]])
                A_free[z] = t_
                tk[("s6", u)] = t_

            def S8(u):
                z = u % 3
                U = units[u]; b_ = U["p"] % 2; og = state["ogrp"] % 2
                t_ = op("pe", lambda e: e.matmul(Oacc[og][:], lhsT=Vs[b_][:, U["k"], U["hh"], :], rhs=Pb2[z][:], start=U["ofirst"], stop=U["olast"]),
                        [tk[("s6", u)], vl[U["p"]]] + ([O_free[og]] if U["ofirst"] else []))
                P_free[z] = t_
                if U["olast"]:
                    epilogue(U, og, t_)
                    state["ogrp"] += 1
                    if U["i"] == NSLOT - 1:
                        pfree[b_] = t_
                        vfree[0] = t_
                        if U["p"] + 1 < 4:
                            load_v(U["p"] + 1)
                        if U["p"] + 2 < 4:
                            pl[U["p"] + 2] = load_pair(U["p"] + 2)

            def epilogue(U, og, done):
                q0 = U["i"] * 512
                a1 = op("act", lambda e: e.activation(out=sq_s[:], in_=Oacc[og][:], func=AF.Square), [done, state["sq_free"]])
                a2 = op("pe", lambda e: e.matmul(ssqb[:], lhsT=blk_f[:], rhs=sq_s[:], start=True, stop=True), [a1, state["ssq_free"], cdone])
                state["sq_free"] = a2
                a3 = op("act", lambda e: e.activation(out=rs_s[:], in_=ssqb[:], func=AF.Ln, scale=1.0 / 64, bias=EPS), [a2, state["rs_free"]])
                state["ssq_free"] = a3
                a4 = op("act", lambda e: e.activation(out=rs_s[:], in_=rs_s[:], func=AF.Exp, scale=-0.5), [a3])
                a5 = op("dve", lambda e: e.scalar_tensor_tensor(out=OT[:, 4 + U["p"], q0:q0 + 512], in0=Oacc[og][:], scalar=gsb_c[:, 0:1], in1=rs_s[:], op0=ALU.mult, op1=ALU.mult), [a4, cdone])
                state["rs_free"] = a5
                O_free[og] = a5

            def dummy():
                op("pe", lambda e: e.matmul(ssqb[:], lhsT=ones_bf[:], rhs=sbm[:, 0, :], start=True, stop=True, skip_group_check=True), [state["ssq_free"], mload, cdone], sig=False)

            S1(0)
            zevery = max(1, (NU - 40) // (CAP // rows_per + 1))
            for r in range(NU + 4):
                if r >= 20 and r % zevery == 0:
                    zero_fill_step(tk.get(("s2", min(r - 1, NU - 1))))
                if 0 <= r < NU: S2(r)
                if 0 <= r - 1 < NU: S3(r - 1)
                if 0 <= r - 3 < NU: S8(r - 3)
                if 0 <= r - 2 < NU: S7(r - 2)
                if 0 <= r - 2 < NU: S6(r - 2)
                if 0 <= r + 1 < NU: S1(r + 1)
                for _ in range(SB_DUMMY_A):
                    dummy()
                if 0 <= r - 1 < NU:
                    S5a(r - 1)
                    S4(r - 1)
                    S5(r - 1)
                for _ in range(SB_DUMMY_B):
                    dummy()
            while zstate["next"] < CAP:
                zero_fill_step()
            ztok = zstate["tok"]
            kb.drain_all()
        nc.all_engine_barrier()

        I32 = mybir.dt.int32
        IOA = bass.IndirectOffsetOnAxis
        with ExitStack() as ph:
            sbA = lambda name, shape, dt: ph.enter_context(nc.sbuf_tensor(name, list(shape), dt))
            psA = lambda name, shape, dt: ph.enter_context(nc.psum_tensor(name, list(shape), dt))
            gffn_b = sbA("gffn_b", [128, D], F32)
            gfs = kb.dsem("gffn")
            gffn_ready = dma("sp", gfs, gffn_b[:], bc_row(g_ffn, D))
            Wo = sbA("Wo", [128, NCH, D], BF16)
            Wr = sbA("Wr", [128, NCH, 36], F32)
            wst = [sbA(f"wstD{i}", [128, D], F32) for i in range(2)]
            xr = [sbA(f"xr{i}", [128, D], F32) for i in range(2)]
            x1 = [sbA(f"x1_{i}", [128, D], F32) for i in range(2)]
            junk = sbA("junkD", [128, D], BF16)
            hf2 = [sbA(f"hf_{i}", [128, D], F32) for i in range(2)]
            statf = [sbA(f"statf{i}", [128, 4], F32) for i in range(2)]
            junkf = [sbA(f"junkf{i}", [128, D], BF16) for i in range(2)]
            hb_ = [sbA(f"hb{i}", [128, D], BF16) for i in range(2)]
            NHB = 8
            hbl = [sbA(f"hbl{i}", [128, D], BF16) for i in range(NHB)]
            hTf = sbA("hTf", [128, NCH, 128], F32)
            rt = sbA("rt", [128, 256], F32)
            stat = sbA("statD", [128, 16], F32)
            G_all = sbA("G_all", [128, NOT_, 32], F32)
            SELb = sbA("SELb", [128, NOT_, 32], BF16)
            SELf = sbA("SELf", [128, NOT_, 32], F32)
            SEL1 = sbA("SEL1", [128, NOT_, 32], F32)
            POS = sbA("POS", [128, NOT_, 32], F32)
            strict_s = sbA("strict_s", [128, 128], BF16)
            thr_s = sbA("thr_s", [128, NB], F32)
            base_s = sbA("base_s", [128, 8], F32)
            big = sbA("bigtmp", [128, max(NOT_, NB) * 32], F32)
            pA = [psA(f"pA{i}", [128, 512], F32) for i in range(4)]
            tpF = psA("tpF", [128, 512], F32)
            pR = psA("pR", [128, 512], F32)

            cs2 = kb.dsem("constD")
            a = dma("sp", cs2, strict_s[:], c_strict.ap())
            a = dma("sp", cs2, thr_s[:], c_thr.ap())
            a = dma("sp", cs2, base_s[:], c_base13.ap())
            cD = a

            wsem = [kb.dsem("wD0"), kb.dsem("wD1")]
            wfree = [None, None]
            wt = None
            for c in range(NCH):
                b_ = c % 2
                ld = dma("sp", wsem[b_], wst[b_][:], w_o.ap()[c * 128:(c + 1) * 128, :], [wfree[b_]])
                wt = op("dve", lambda e, b_=b_, c=c: e.tensor_copy(out=Wo[:, c, :], in_=wst[b_][:]), [ld])
                wfree[b_] = wt
            Wo_ready = wt
            rs_ = kb.dsem("wr")
            a = dma("sp", rs_, Wr[:, :, 0:4], w_rg.ap().rearrange("(c p) n -> p c n", p=128))
            a = dma("sp", rs_, Wr[:, :, 4:36], w_re.ap().rearrange("(c p) n -> p c n", p=128))
            Wr_ready = a

            xsem = [kb.dsem("xr0"), kb.dsem("xr1")]
            osem = [kb.dsem("x1w0"), kb.dsem("x1w1")]
            x1w_toks = [None, None]
            xr_free = [None, None]; x1_free = [None, None]
            pA_free = [None] * 4
            dst = {"tpF_free": None, "hf_free": None, "hTf_free": None, "rt_free": None, "stat_free": None}
            hbsem = [kb.dsem("hbw0"), kb.dsem("hbw1")]
            hbw = [None, None]
            tile_ctx = {}
            hf_free = [None, None]; statf_free = [None, None]

            def front(n):
                b_ = n % 2
                r0 = n * 128
                ld = dma("sp", xsem[b_], xr[b_][:], xq.ap()[r0:r0 + 128, :], [xr_free[b_]])
                hs = []
                for half in range(2):
                    i = (2 * n + half) % 4
                    mm = None
                    for c in range(NCH):
                        mm = op("pe", lambda e, c=c, i=i, half=half: e.matmul(pA[i][:], lhsT=OT[:, c, r0:r0 + 128], rhs=Wo[:, c, half * 512:(half + 1) * 512], start=(c == 0), stop=(c == NCH - 1)),
                                [Wo_ready, pA_free[i]] if c == 0 else [], sig=(c == NCH - 1))
                    ad = op("dve", lambda e, i=i, half=half: e.tensor_tensor(out=x1[b_][:, half * 512:(half + 1) * 512], in0=pA[i][:], in1=xr[b_][:, half * 512:(half + 1) * 512], op=ALU.add), [mm, ld, x1_free[b_]])
                    pA_free[i] = ad
                    hs.append(ad)
                xr_free[b_] = hs[1]
                x1w = dma("sp", osem[b_], X1_sc.ap()[r0:r0 + 128, :], x1[b_][:], hs)
                x1w_toks[b_] = x1w
                sq = op("act", lambda e: e.activation(out=junkf[b_][:], in_=x1[b_][:], func=AF.Square, accum_out=statf[b_][:, 0:1]), hs + [statf_free[b_]])
                r2 = rsqrt(statf[b_][:, 2:3], statf[b_][:, 0:1], D, [sq])
                hfo = op("dve", lambda e: e.scalar_tensor_tensor(out=hf2[b_][:], in0=x1[b_][:], scalar=statf[b_][:, 2:3], in1=gffn_b[:], op0=ALU.mult, op1=ALU.mult), [r2, hf_free[b_], gffn_ready])
                statf_free[b_] = hfo
                x1_free[b_] = x1w
                hbo = op("dve", lambda e: e.tensor_copy(out=hb_[b_][:], in_=hf2[b_][:]), [hfo, hbw[b_]])
                hbw[b_] = dma("sp", hbsem[b_], HB_sc.ap()[r0:r0 + 128, :], hb_[b_][:], [hbo])
                tile_ctx[n] = dict(hfo=hfo, hbo=hbo, r2=r2)

            def mid_(n):
                b_ = n % 2
                hfo = tile_ctx[n]['hfo']; hbo = tile_ctx[n]['hbo']; r2 = tile_ctx[n]['r2']
                tps = []
                tp2 = None
                for grp in range(2):
                    for c4 in range(4):
                        c = grp * 4 + c4
                        tp2 = op("pe", lambda e, c=c, c4=c4: e.transpose(tpF[:, c4 * 128:(c4 + 1) * 128], hf2[b_][:, c * 128:(c + 1) * 128], ident_f[:]), [hfo, dst["tpF_free"]] if c4 == 0 else [], sig=(c4 == 3))
                    cpf = op("act", lambda e, grp=grp: e.copy(out=hTf[:, grp * 4:(grp + 1) * 4, :], in_=tpF[:].rearrange("p (a t) -> p a t", t=128)), [tp2] + ([dst["hTf_free"]] if grp == 0 else []))
                    dst["tpF_free"] = cpf
                    tps.append(cpf)
                hf_free[b_] = tp2
                stat_prev = dst["rt_free"]
                lg = None
                for c in range(NCH):
                    lg = op("pe", lambda e, c=c: e.matmul(tpF[:, 0:36], lhsT=hTf[:, c, :], rhs=Wr[:, c, :], start=(c == 0), stop=(c == NCH - 1)), [tps[1], Wr_ready] if c == 0 else [], sig=(c == NCH - 1))
                dst["hTf_free"] = lg
                tile_ctx[n]['lg'] = lg; tile_ctx[n]['stat_prev'] = stat_prev

            def back(n):
                b_ = n % 2
                hfo = tile_ctx[n]['hfo']; hbo = tile_ctx[n]['hbo']; r2 = tile_ctx[n]['r2']; lg = tile_ctx[n]['lg']; stat_prev = tile_ctx[n]['stat_prev']
                q1 = op("dve", lambda e: e.tensor_tensor(out=rt[:, 0:36], in0=tpF[:, 0:36], in1=rbias_b[:], op=ALU.add), [lg, stat_prev, cdone, hbo])
                dst["tpF_free"] = q1
                q2 = op("dve", lambda e: e.tensor_reduce(out=stat[:, 4:5], in_=rt[:, 0:4], axis=AX.X, op=ALU.max), [q1, r2, hfo])
                q3 = op("dve", lambda e: e.tensor_scalar(out=rt[:, 40:44], in0=rt[:, 0:4], scalar1=stat[:, 4:5], scalar2=None, op0=ALU.is_ge), [q2])
                q4 = op("dve", lambda e: e.tensor_scalar(out=stat[:, 5:6], in0=stat[:, 4:5], scalar1=-1.0, scalar2=None, op0=ALU.mult), [q2])
                q5 = op("act", lambda e: e.activation(out=rt[:, 180:184], in_=rt[:, 0:4], func=AF.Exp, bias=stat[:, 5:6], accum_out=stat[:, 6:7]), [q4])
                q6 = op("dve", lambda e: e.reciprocal(out=stat[:, 7:8], in_=stat[:, 6:7]), [q5])
                q7 = op("dve", lambda e: e.tensor_scalar(out=rt[:, 44:48], in0=rt[:, 40:44], scalar1=1.0, scalar2=BIG, op0=ALU.subtract, op1=ALU.mult), [q3])
                q8 = op("dve", lambda e: e.tensor_tensor(out=rt[:, 48:80].rearrange("p (g j) -> p g j", j=8), in0=rt[:, 4:36].rearrange("p (g j) -> p g j", j=8),
                                                         in1=rt[:, 44:48].unsqueeze(2).to_broadcast([128, 4, 8]), op=ALU.add), [q7])
                q9 = op("dve", lambda e: e.tensor_reduce(out=stat[:, 8:9], in_=rt[:, 48:80], axis=AX.X, op=ALU.max), [q8])
                q10 = op("dve", lambda e: e.tensor_scalar(out=rt[:, 80:112], in0=rt[:, 48:80], scalar1=stat[:, 8:9], scalar2=None, op0=ALU.is_ge), [q9])
                q11 = op("dve", lambda e: e.scalar_tensor_tensor(out=rt[:, 184:216], in0=rt[:, 80:112], scalar=-BIG, in1=rt[:, 48:80], op0=ALU.mult, op1=ALU.add), [q10])
                q12 = op("dve", lambda e: e.tensor_reduce(out=stat[:, 9:10], in_=rt[:, 184:216], axis=AX.X, op=ALU.max), [q11])
                q13 = op("dve", lambda e: e.tensor_scalar(out=rt[:, 112:144], in0=rt[:, 48:80], scalar1=stat[:, 9:10], scalar2=None, op0=ALU.is_ge), [q12])
                q14 = op("dve", lambda e: e.tensor_scalar(out=stat[:, 10:11], in0=stat[:, 8:9], scalar1=-1.0, scalar2=None, op0=ALU.mult), [q9])
                q15 = op("act", lambda e: e.activation(out=rt[:, 144:176], in_=rt[:, 48:80], func=AF.Exp, bias=stat[:, 10:11]), [q14, q8])
                q16 = op("act", lambda e: e.activation(out=stat[:, 11:12], in_=stat[:, 9:10], func=AF.Exp, bias=stat[:, 10:11]), [q14, q12])
                q17 = op("dve", lambda e: e.tensor_scalar(out=stat[:, 12:13], in0=stat[:, 11:12], scalar1=1.0, scalar2=None, op0=ALU.add), [q16])
                q18 = op("dve", lambda e: e.reciprocal(out=stat[:, 13:14], in_=stat[:, 12:13]), [q17])
                q19 = op("dve", lambda e: e.tensor_tensor(out=stat[:, 14:15], in0=stat[:, 13:14], in1=stat[:, 7:8], op=ALU.mult), [q18, q6])
                q20 = op("dve", lambda e: e.scalar_tensor_tensor(out=G_all[:, n, :], in0=rt[:, 144:176], scalar=stat[:, 14:15], in1=rt[:, 112:144], op0=ALU.mult, op1=ALU.mult), [q19, q15, q13])
                q21 = op("dve", lambda e: e.tensor_copy(out=SELb[:, n, :], in_=rt[:, 112:144]), [q13])
                q22 = op("dve", lambda e: e.tensor_copy(out=SELf[:, n, :], in_=rt[:, 112:144]), [q13])
                q23 = op("dve", lambda e: e.tensor_copy(out=SEL1[:, n, :], in_=rt[:, 80:112]), [q10])
                dst["rt_free"] = q23
                dst["stat_free"] = q23

            front(0)
            for n in range(NOT_):
                if n + 1 < NOT_:
                    front(n + 1)
                mid_(n)
                back(n)
            router_done = dst["rt_free"]

            ptok = None
            for n in range(NOT_):
                col = (n % 8) * 32
                mm = None
                for n2 in range(n):
                    mm = op("pe", lambda e, n2=n2, col=col: e.matmul(pR[:, col:col + 32], lhsT=ones_bf[:], rhs=SELb[:, n2, :], start=(n2 == 0), stop=False, skip_group_check=True),
                            [router_done, cD, ptok] if n2 == 0 else [], sig=False)
                mm = op("pe", lambda e, n=n, col=col: e.matmul(pR[:, col:col + 32], lhsT=strict_s[:], rhs=SELb[:, n, :], start=(n == 0), stop=True, skip_group_check=True), [router_done, cD, ptok])
                ptok = op("dve", lambda e, n=n, col=col: e.tensor_copy(out=POS[:, n, :], in_=pR[:, col:col + 32]), [mm])
            mm = None
            for n2 in range(NOT_):
                mm = op("pe", lambda e, n2=n2: e.matmul(tpF[:, 64:96], lhsT=ones_bf[:], rhs=SELb[:, n2, :], start=(n2 == 0), stop=(n2 == NOT_ - 1), skip_group_check=True), [router_done] if n2 == 0 else [], sig=(n2 == NOT_ - 1))
            w1_ = op("dve", lambda e: e.tensor_copy(out=rt[:, 0:32], in_=tpF[:, 64:96]), [mm, ptok])
            NJ = 2 * NOWN // MOE_B
            w2_ = op("dve", lambda e: e.tensor_tensor(out=big[:, 0:32 * NJ].rearrange("p (e j) -> p e j", j=NJ), in0=rt[:, 0:32].unsqueeze(2).to_broadcast([128, 32, NJ]),
                                                      in1=thr_s[:, 0:NJ].unsqueeze(1).to_broadcast([128, 32, NJ]), op=ALU.is_gt), [w1_, cD])
            w3_ = op("dve", lambda e: e.tensor_reduce(out=rt[:, 32:64], in_=big[:, 0:32 * NJ].rearrange("p (e j) -> p e j", j=NJ), axis=AX.X, op=ALU.add), [w2_])
            w5_ = op("dve", lambda e: e.tensor_scalar(out=rt[:, 64:96], in0=rt[:, 32:64], scalar1=float(MOE_B), scalar2=None, op0=ALU.mult), [w3_])
            prev = op("dve", lambda e: e.tensor_copy(out=rt[:, 96:128], in_=rt[:, 64:96]), [w5_])
            src, dstc = 96, 128
            k = 1
            while k < 32:
                a1 = op("dve", lambda e, src=src, dstc=dstc, k=k: e.tensor_copy(out=rt[:, dstc:dstc + k], in_=rt[:, src:src + k]), [prev])
                prev = op("dve", lambda e, src=src, dstc=dstc, k=k: e.tensor_tensor(out=rt[:, dstc + k:dstc + 32], in0=rt[:, src + k:src + 32], in1=rt[:, src:src + 32 - k], op=ALU.add), [a1])
                src, dstc = dstc, src
                k *= 2
            pe_ = op("dve", lambda e, src=src: e.tensor_copy(out=rt[:, 160:192], in_=rt[:, src:src + 32]), [prev])
            ps_ = op("dve", lambda e: e.tensor_tensor(out=rt[:, 192:224], in0=rt[:, 160:192], in1=rt[:, 64:96], op=ALU.subtract), [pe_])
            dsum = sbA("dsum", [128, NOT_, 4], F32)
            bigv = big[:, 0:NOT_ * 32].rearrange("p (n e) -> p n e", e=32)
            v1 = op("dve", lambda e: e.tensor_tensor(out=POS[:], in0=POS[:], in1=rt[:, 192:224].unsqueeze(1).to_broadcast([128, NOT_, 32]), op=ALU.add), [ps_])
            v2 = op("dve", lambda e: e.tensor_tensor(out=bigv, in0=POS[:], in1=SEL1[:], op=ALU.mult), [v1])
            v3 = op("dve", lambda e: e.tensor_reduce(out=dsum[:, :, 0:1], in_=bigv, axis=AX.X, op=ALU.add), [v2])
            v4 = op("dve", lambda e: e.tensor_tensor(out=bigv, in0=POS[:], in1=SELf[:], op=ALU.mult), [v3])
            v5 = op("dve", lambda e: e.tensor_reduce(out=dsum[:, :, 1:2], in_=bigv, axis=AX.X, op=ALU.add), [v4])
            v6 = op("dve", lambda e: e.tensor_tensor(out=dsum[:, :, 1:2], in0=dsum[:, :, 1:2], in1=dsum[:, :, 0:1], op=ALU.subtract), [v5])
            v7 = op("dve", lambda e: e.tensor_copy(out=D12i[:], in_=dsum[:, :, 0:2]), [v6])
            v8 = op("dve", lambda e: e.tensor_tensor(out=bigv, in0=G_all[:], in1=SEL1[:], op=ALU.mult), [v7])
            v9 = op("dve", lambda e: e.tensor_reduce(out=GT[:, :, 0:1], in_=bigv, axis=AX.X, op=ALU.add), [v8])
            v10 = op("dve", lambda e: e.tensor_reduce(out=GT[:, :, 1:2], in_=G_all[:], axis=AX.X, op=ALU.add), [v9])
            v11 = op("dve", lambda e: e.tensor_tensor(out=GT[:, :, 1:2], in0=GT[:, :, 1:2], in1=GT[:, :, 0:1], op=ALU.subtract), [v10])
            dest_ready = v11
            bigb = big[:, 0:NB * 32].rearrange("p (b e) -> p b e", e=32)
            bex = sbA("bex", [128, NB], F32)
            idxf = sbA("idxf", [128, NB], F32)
            u1 = op("dve", lambda e: e.tensor_tensor(out=bigb, in0=rt[:, 160:192].unsqueeze(1).to_broadcast([128, NB, 32]), in1=thr_s[:].unsqueeze(2).to_broadcast([128, NB, 32]), op=ALU.is_le), [v11, cD])
            u2 = op("dve", lambda e: e.tensor_reduce(out=bex[:], in_=bigb, axis=AX.X, op=ALU.add), [u1])
            u3 = op("dve", lambda e: e.tensor_scalar(out=bex[:], in0=bex[:], scalar1=float(NEXP - 1), scalar2=None, op0=ALU.min), [u2])
            u4 = op("dve", lambda e: e.tensor_scalar(out=idxf[:], in0=bex[:], scalar1=128.0, scalar2=base_s[:, 0:1], op0=ALU.mult, op1=ALU.add), [u3])
            u7 = op("dve", lambda e: e.tensor_copy(out=idxwi[:], in_=idxf[:]), [u4])
            idx_ready = u7

            scs = [kb.dsem(f"scat{i}") for i in range(NHB)]
            lss = [kb.dsem(f"hbl{i}") for i in range(NHB)]

            def hb_load(n):
                b_ = n % NHB
                dma("sp", lss[b_], hbl[b_][:], HB_sc.ap()[n * 128:(n + 1) * 128, :], [(scs[b_], scs[b_].count)] + hbw)

            for n in range(min(NHB, NOT_)):
                hb_load(n)
            kb.wait("pool", [dest_ready, ztok])
            for n in range(NOT_):
                b_ = n % NHB
                kb.wait("pool", [(lss[b_], lss[b_].count)])
                for k2 in range(2):
                    ins = nc.gpsimd.indirect_dma_start(out=XS_sc.ap(), out_offset=IOA(ap=D12i[:, n, k2:k2 + 1], axis=0), in_=hbl[b_][:], in_offset=None)
                    scs[b_].count += 16
                    ins.then_inc(scs[b_].h, 16)
                if n - 4 >= 0 and n - 4 + NHB < NOT_:
                    hb_load(n - 4 + NHB)
            kb.wait("pool", [(s_, s_.count) for s_ in scs])
            scat_done = op("pool", lambda e: e.memset(hbl[0][0:1, 0:2], 0.0))
            kb.wait("sp", x1w_toks + [scat_done])
            kb.drain_all()
        mid.close()
        nc.all_engine_barrier()

        with ExitStack() as ph:
            sbA = lambda name, shape, dt: ph.enter_context(nc.sbuf_tensor(name, list(shape), dt))
            psA = lambda name, shape, dt: ph.enter_context(nc.psum_tensor(name, list(shape), dt))
            W1 = [sbA(f"W1_{i}", [128, NCH, HID], BF16) for i in range(2)]
            W3 = [sbA(f"W3_{i}", [128, NCH, HID], BF16) for i in range(2)]
            W2 = [sbA(f"W2_{i}", [128, 4, D], BF16) for i in range(2)]
            Wst1 = [sbA(f"Wst1_{i}", [128, NCH, HID], F32) for i in range(2)]
            Wst3 = [sbA(f"Wst3_{i}", [128, NCH, HID], F32) for i in range(2)]
            Wst2 = [sbA(f"Wst2_{i}", [128, 4, D], F32) for i in range(2)]
            xblk = [sbA(f"xblk{i}", [128, 4, D], BF16) for i in range(2)]
            xT = [sbA(f"xTm{i}", [128, NCH, 512], BF16) for i in range(2)]
            sil = [sbA(f"sil{i}", [128, 512], BF16) for i in range(2)]
            actT = [sbA(f"actT{i}", [128, 4, 512], BF16) for i in range(2)]
            ysb1 = sbA("ysb", [128, 4, D], F32)
            ysb = [ysb1, ysb1]
            pA = [psA(f"pE{i}", [128, 512], F32) for i in range(4)]
            pY = [psA(f"pY{i}", [128, 512], F32) for i in range(2)]
            tpX = [psA(f"tpX{i}", [128, NCH, 128], BF16) for i in range(2)]
            pA_free = [None] * 4; pY_free = [None, None]; tpX_free = [None, None]
            xls = [kb.dsem("xsL0"), kb.dsem("xsL1")]
            yws = [kb.dsem("ysW0"), kb.dsem("ysW1")]
            w_free = [None, None]; xblk_free = [None, None]; xT_free = [None, None]; sil_free = [None, None]; act_free = [None, None]
            ysb_free = [None, None]
            yw_toks = [None, None]
            w1v = w1.ap().rearrange("e (p c) n -> (e p) (c n)", c=NCH)
            w3v = w3.ap().rearrange("e (p c) n -> (e p) (c n)", c=NCH)
            w2v = w2.ap().rearrange("e (p c) n -> (e p) (c n)", c=4)
            ntp = 0
            gsem2 = [kb.dsem("moeWst0"), kb.dsem("moeWst1")]
            st_free = [None, None]
            conv = {}
            gtoks = {}

            def gather_w(blk):
                sb_ = blk % 2
                kb.wait("pool", [st_free[sb_], idx_ready])
                for (dst_, src_) in ((Wst1[sb_], w1v), (Wst3[sb_], w3v), (Wst2[sb_], w2v)):
                    ins = nc.gpsimd.indirect_dma_start(out=dst_[:].rearrange("p c n -> p (c n)"), out_offset=None, in_=src_, in_offset=IOA(ap=idxwi[:, blk:blk + 1], axis=0))
                    gsem2[sb_].count += 16; ins.then_inc(gsem2[sb_].h, 16)
                gtoks[blk] = (gsem2[sb_], gsem2[sb_].count)

            def convert_w(blk):
                wb_ = blk % 2
                gtok = gtoks[blk]
                c1 = op("act", lambda e: e.copy(out=W1[wb_][:], in_=Wst1[wb_][:]), [gtok, w_free[wb_]])
                c2 = op("dve", lambda e: e.tensor_copy(out=W3[wb_][:], in_=Wst3[wb_][:]), [gtok, w_free[wb_]])
                c3 = op("act", lambda e: e.copy(out=W2[wb_][:, 0:2, :], in_=Wst2[wb_][:, 0:2, :]), [gtok])
                c4 = op("dve", lambda e: e.tensor_copy(out=W2[wb_][:, 2:4, :], in_=Wst2[wb_][:, 2:4, :]), [gtok])
                conv[blk] = [c3, c4]
                st_free[wb_] = None
                kb.wait("pool", [c3, c4])
                if blk + 2 < NB:
                    gather_w(blk + 2)

            gather_w(0)
            if NB > 1:
                gather_w(1)
            convert_w(0)
            for blk in range(NB):
                wb = blk % 2
                wready = None
                if blk == 0:
                    xl_next = dma("sp", xls[0], xblk[0][:], XS_sc.ap()[0:MOE_B, :].rearrange("(t p) d -> p t d", p=128), [scat_done])
                xl = xl_next
                if blk + 1 < NB:
                    xl_next = dma("sp", xls[1 - wb], xblk[1 - wb][:], XS_sc.ap()[(blk + 1) * MOE_B:(blk + 2) * MOE_B, :].rearrange("(t p) d -> p t d", p=128), [xblk_free[1 - wb], scat_done])
                cpx = None
                for tt in range(4):
                    tb = ntp % 2; ntp += 1
                    tp = None
                    for c in range(NCH):
                        tp = op("pe", lambda e, c=c, tt=tt, tb=tb: e.transpose(tpX[tb][:, c, :], xblk[wb][:, tt, c:D:NCH], ident_bf[:]), [xl, tpX_free[tb]] if c == 0 else [], sig=(c == NCH - 1))
                    cpx = op("act" if tt % 2 == 0 else "dve",
                             (lambda e, tt=tt, tb=tb: e.copy(out=xT[wb][:, :, tt * 128:(tt + 1) * 128], in_=tpX[tb][:])) if tt % 2 == 0 else
                             (lambda e, tt=tt, tb=tb: e.tensor_copy(out=xT[wb][:, :, tt * 128:(tt + 1) * 128], in_=tpX[tb][:])),
                             [tp] + ([xT_free[wb]] if tt < 2 else []))
                    tpX_free[tb] = cpx
                    xT_ready_prev = cpx
                xblk_free[wb] = (kb.esem["pe"], kb.esem["pe"].count)
                xT_ready = [(kb.esem["act"], kb.esem["act"].count), (kb.esem["dve"], kb.esem["dve"].count)]
                ab = blk % 2
                s2 = None
                for hc in range(4):
                    i1 = (2 * hc) % 4; i3 = (2 * hc + 1) % 4
                    m1 = None
                    for c in range(NCH):
                        m1 = op("pe", lambda e, c=c, hc=hc, i1=i1: e.matmul(pA[i1][:], lhsT=W1[wb][:, c, hc:HID:4], rhs=xT[wb][:, c, :], start=(c == 0), stop=(c == NCH - 1)),
                                (conv[blk] + [pA_free[i1]] + xT_ready) if c == 0 else [], sig=(c == NCH - 1))
                    m3 = None
                    for c in range(NCH):
                        m3 = op("pe", lambda e, c=c, hc=hc, i3=i3: e.matmul(pA[i3][:], lhsT=W3[wb][:, c, hc:HID:4], rhs=xT[wb][:, c, :], start=(c == 0), stop=(c == NCH - 1)),
                                [pA_free[i3]] if c == 0 else [], sig=(c == NCH - 1))
                    sb_i = hc % 2
                    s1 = op("act", lambda e, i1=i1, sb_i=sb_i: e.activation(out=sil[sb_i][:], in_=pA[i1][:], func=AF.Silu), [m1, sil_free[sb_i]])
                    pA_free[i1] = s1
                    s2 = op("dve", lambda e, i3=i3, sb_i=sb_i, hc=hc, ab=ab: e.tensor_tensor(out=actT[ab][:, hc, :], in0=pA[i3][:], in1=sil[sb_i][:], op=ALU.mult), [m3, s1] + ([act_free[ab]] if hc == 0 else []))
                    pA_free[i3] = s2
                    sil_free[sb_i] = s2
                xT_free[wb] = (kb.esem["pe"], kb.esem["pe"].count)
                a_ready = s2
                if blk + 1 < NB:
                    convert_w(blk + 1)
                lastpe = None
                ycp = None
                for tt in range(4):
                    for half in range(2):
                        yb = (tt * 2 + half) % 2
                        my = None
                        for hc in range(4):
                            my = op("pe", lambda e, hc=hc, tt=tt, half=half, yb=yb, ab=ab: e.matmul(pY[yb][:], lhsT=actT[ab][:, hc, tt * 128:(tt + 1) * 128], rhs=W2[wb][:, hc, half * 512:(half + 1) * 512], start=(hc == 0), stop=(hc == 3)),
                                    [a_ready, pY_free[yb]] if hc == 0 else [], sig=(hc == 3))
                        if half == 0:
                            ycp = op("act", lambda e, yb=yb, tt=tt, half=half: e.copy(out=ysb[wb][:, tt, half * 512:(half + 1) * 512], in_=pY[yb][:]), [my] + ([ysb_free[wb]] if tt == 0 else []))
                        else:
                            ycp = op("dve", lambda e, yb=yb, tt=tt, half=half: e.tensor_copy(out=ysb[wb][:, tt, half * 512:(half + 1) * 512], in_=pY[yb][:]), [my] + ([ysb_free[wb]] if tt == 0 else []))
                        pY_free[yb] = ycp
                        lastpe = my
                act_free[ab] = lastpe
                w_free[wb] = lastpe
                yw = dma("sp", yws[wb], YS_sc.ap()[blk * MOE_B:(blk + 1) * MOE_B, :].rearrange("(t p) d -> p t d", p=128), ysb[wb][:],
                         [(kb.esem["act"], kb.esem["act"].count), (kb.esem["dve"], kb.esem["dve"].count)])
                ysb_free[0] = yw; ysb_free[1] = yw
                yw_toks[wb] = yw
            kb.wait("sp", yw_toks)
            kb.wait("pool", yw_toks)
            kb.drain_all()
        nc.all_engine_barrier()

        with ExitStack() as ph:
            sbA = lambda name, shape, dt: ph.enter_context(nc.sbuf_tensor(name, list(shape), dt))
            xo = [sbA(f"xo{i}", [128, D], F32) for i in range(4)]
            y1 = [sbA(f"y1_{i}", [128, D], F32) for i in range(4)]
            y2 = [sbA(f"y2_{i}", [128, D], F32) for i in range(4)]
            xls = [kb.dsem(f"x1L{i}") for i in range(4)]
            gs_ = [kb.dsem(f"yG{i}") for i in range(4)]
            for s_ in gs_:
                kb.dma_sems.remove(s_)
            ows = [kb.dsem(f"oW{i}") for i in range(4)]
            xo_free = [None] * 4; y_free = [None] * 4
            ow = [None] * 4
            for n in range(NOT_):
                b_ = n % 4
                r0 = n * 128
                ld = dma("sp", xls[b_], xo[b_][:], X1_sc.ap()[r0:r0 + 128, :], [xo_free[b_]])
                kb.wait("pool", [y_free[b_]])
                ins = nc.gpsimd.indirect_dma_start(out=y1[b_][:], out_offset=None, in_=YS_sc.ap(), in_offset=IOA(ap=D12i[:, n, 0:1], axis=0))
                gs_[b_].count += 16; ins.then_inc(gs_[b_].h, 16)
                ins = nc.gpsimd.indirect_dma_start(out=y2[b_][:], out_offset=None, in_=YS_sc.ap(), in_offset=IOA(ap=D12i[:, n, 1:2], axis=0))
                gs_[b_].count += 16; ins.then_inc(gs_[b_].h, 16)
                gt = (gs_[b_], gs_[b_].count)
                c1 = op("dve", lambda e: e.scalar_tensor_tensor(out=xo[b_][:], in0=y1[b_][:], scalar=GT[:, n, 0:1], in1=xo[b_][:], op0=ALU.mult, op1=ALU.add), [ld, gt])
                c2 = op("dve", lambda e: e.scalar_tensor_tensor(out=xo[b_][:], in0=y2[b_][:], scalar=GT[:, n, 1:2], in1=xo[b_][:], op0=ALU.mult, op1=ALU.add), [c1])
                y_free[b_] = c2
                ow[b_] = dma("sp", ows[b_], out.ap()[r0:r0 + 128, :], xo[b_][:], [c2])
                xo_free[b_] = ow[b_]
            kb.wait("sp", ow)
            kb.dma_sems.extend(gs_)
            kb.drain_all()
    return nc


_CACHE = {}


def kernel(**inputs):
    x = np.asarray(inputs["x"], dtype=np.float32)
    B, S, _ = x.shape
    ncores = 2 * B
    if S not in _CACHE:
        _CACHE[S] = build_nc(S)
    nc = _CACHE[S]
    NSLOT = S // 1024
    consts = [host_constants(hf, (S + NEXP * MOE_B) // MOE_B) for hf in range(2)]
    in_maps = []
    own_idx = []
    for c in range(ncores):
        b, hf = c // 2, c % 2
        idx = np.concatenate([np.arange(512 * (2 * i + hf), 512 * (2 * i + hf) + 512) for i in range(NSLOT)])
        own_idx.append(idx)
        m = {"xb": np.ascontiguousarray(x[b]), "xq": np.ascontiguousarray(x[b][idx])}
        for k in ("g_attn", "w_in", "qn_g", "kn_g", "lam_q1", "lam_k1", "lam_q2", "lam_k2", "subln_g", "sb_out_g",
                  "w_o", "g_ffn", "w_router_g", "b_router_g", "w_router_e", "b_router_e", "w1", "w3", "w2"):
            m[k] = np.ascontiguousarray(np.asarray(inputs[k], dtype=np.float32)[0])
        m["rel_bias"] = np.ascontiguousarray(np.asarray(inputs["rel_bias"], dtype=np.float32))
        m.update(consts[hf])
        in_maps.append(m)
    res = run_bass_kernel_spmd(nc, in_maps, core_ids=list(range(ncores)))
    outp = np.empty((B, S, D), np.float32)
    for c in range(ncores):
        b = c // 2
        outp[b, own_idx[c]] = np.asarray(res.results[c]["out"], dtype=np.float32)
    return outp
```
